# Optimizing a Trainium2 kernel written in Bass

```python
import math
import jax, jax.numpy as jnp
from jax import lax
import numpy as np

D_MODEL = 4096
BATCH = 4
SEQ = 2048
DEPTH = 2

CHUNK = 64
HEAD_DIM = 128
H_A = 16
H_B = 16
W_A = H_A * HEAD_DIM
W_B = H_B * HEAD_DIM
N_IN = 3 * W_A + H_A + 3 * W_B
Q_BLOCK = 128
LEFT_CHUNKS = 8
MAX_REL = 128
N_GROUPS = 4
E_PER_GROUP = 8
TOP_K = 2
D_FF = 512
EPS = 1e-6
NEG_INF = -1e30
ATTN_SCALE = HEAD_DIM ** -0.5

kernel_name = "chunk_causal_fox_relpos_hier_moe_adaln"


def rmsnorm(x, g):
    x32 = x.astype(jnp.float32)
    y = x32 * lax.rsqrt(jnp.mean(x32 * x32, axis=-1, keepdims=True) + EPS)
    return (y * g.astype(jnp.float32)).astype(x.dtype)


def modulate(h, shift, scale):
    return h * (1 + scale[:, None, :]) + shift[:, None, :]


def to_heads(t, n_heads):
    b, s, _ = t.shape
    return t.reshape(b, s, n_heads, HEAD_DIM).transpose(0, 2, 1, 3)


def from_heads(t):
    b, h, s, d = t.shape
    return t.transpose(0, 2, 1, 3).reshape(b, s, h * d)


def forgetting_attention(q, k, v, log_f):
    s_len = q.shape[2]
    cum = jnp.cumsum(log_f, axis=-1)
    outs = []
    for i in range(s_len // Q_BLOCK):
        q0, q1 = i * Q_BLOCK, (i + 1) * Q_BLOCK
        qb = q[:, :, q0:q1]
        kb = k[:, :, :q1]
        vb = v[:, :, :q1]
        s = jnp.einsum('bhqd,bhkd->bhqk', qb, kb).astype(jnp.float32) * ATTN_SCALE
        s = s + (cum[:, :, q0:q1, None] - cum[:, :, None, :q1])
        causal = jnp.arange(q0, q1)[:, None] >= jnp.arange(q1)[None, :]
        s = jnp.where(causal, s, NEG_INF)
        p = jax.nn.softmax(s, axis=-1).astype(v.dtype)
        outs.append(jnp.einsum('bhqk,bhkd->bhqd', p, vb))
    return jnp.concatenate(outs, axis=2)


def chunked_relpos_attention(q, k, v, rel_table):
    b, h, s_len, d = q.shape
    n_chunks = s_len // CHUNK
    pad = LEFT_CHUNKS * CHUNK
    band = (LEFT_CHUNKS + 1) * CHUNK
    k_pad = jnp.pad(k, ((0, 0), (0, 0), (pad, 0), (0, 0)))
    v_pad = jnp.pad(v, ((0, 0), (0, 0), (pad, 0), (0, 0)))
    qi = jnp.arange(CHUNK)
    kj = jnp.arange(band)
    dist = qi[:, None] - kj[None, :] + pad
    rel_idx = jnp.clip(dist, -MAX_REL, MAX_REL) + MAX_REL
    bias = rel_table.astype(jnp.float32)[:, rel_idx]
    q_chunks = q.reshape(b, h, n_chunks, CHUNK, d).transpose(2, 0, 1, 3, 4)

    def one_chunk(args):
        n, qc = args
        start = n * CHUNK
        kb = lax.dynamic_slice_in_dim(k_pad, start, band, axis=2)
        vb = lax.dynamic_slice_in_dim(v_pad, start, band, axis=2)
        s = jnp.einsum('bhqd,bhkd->bhqk', qc, kb).astype(jnp.float32) * ATTN_SCALE
        s = s + bias[None]
        key_pos = start - pad + kj
        s = jnp.where((key_pos >= 0)[None, None, None, :], s, NEG_INF)
        p = jax.nn.softmax(s, axis=-1).astype(vb.dtype)
        return jnp.einsum('bhqk,bhkd->bhqd', p, vb)

    out = lax.map(one_chunk, (jnp.arange(n_chunks), q_chunks))
    return out.transpose(1, 2, 0, 3, 4).reshape(b, h, s_len, d)


def hybrid_mixer(h, w_in, b_forget, rel_bias, w_branch_a, w_branch_b, w_gate, b_gate, w_out):
    proj = h @ w_in
    splits = [W_A, 2 * W_A, 3 * W_A, 3 * W_A + H_A,
              3 * W_A + H_A + W_B, 3 * W_A + H_A + 2 * W_B]
    qa, ka, va, fa, qb, kb, vb = jnp.split(proj, splits, axis=-1)
    log_f = jax.nn.log_sigmoid(fa.astype(jnp.float32) + b_forget.astype(jnp.float32))
    log_f = log_f.transpose(0, 2, 1)
    ya = forgetting_attention(to_heads(qa, H_A), to_heads(ka, H_A), to_heads(va, H_A), log_f)
    yb = chunked_relpos_attention(to_heads(qb, H_B), to_heads(kb, H_B), to_heads(vb, H_B), rel_bias)
    gates = jax.nn.sigmoid(h @ w_gate + b_gate)
    g_a, g_b = jnp.split(gates, 2, axis=-1)
    merged = g_a * (from_heads(ya) @ w_branch_a) + g_b * (from_heads(yb) @ w_branch_b)
    return merged @ w_out


def hierarchical_moe(h, w_rg, b_rg, w_re, b_re, w_eg, w_eu, w_ed):
    b, s, d = h.shape
    t = h.reshape(b * s, d)
    n_tok = t.shape[0]
    g_logits = (t @ w_rg).astype(jnp.float32) + b_rg.astype(jnp.float32)
    g_prob = jax.nn.softmax(g_logits, axis=-1)
    g_sel = jnp.argmax(g_logits, axis=-1)
    g_w = jnp.take_along_axis(g_prob, g_sel[:, None], axis=-1)
    e_logits = ((t @ w_re).astype(jnp.float32) + b_re.astype(jnp.float32))
    e_logits = e_logits.reshape(n_tok, N_GROUPS, E_PER_GROUP)
    e_in = jnp.take_along_axis(e_logits, g_sel[:, None, None], axis=1)[:, 0]
    top_v, top_i = lax.top_k(e_in, TOP_K)
    top_w = jax.nn.softmax(top_v, axis=-1) * g_w
    w_e = jnp.sum(jax.nn.one_hot(top_i, E_PER_GROUP, dtype=jnp.float32) * top_w[..., None], axis=1)
    combine = jax.nn.one_hot(g_sel, N_GROUPS, dtype=jnp.float32)[:, :, None] * w_e[:, None, :]
    out = jnp.zeros((n_tok, d), t.dtype)
    for g in range(N_GROUPS):
        a = jax.nn.silu(jnp.einsum('td,edf->tef', t, w_eg[g])) * jnp.einsum('td,edf->tef', t, w_eu[g])
        a = a * combine[:, g, :, None].astype(a.dtype)
        out = out + jnp.einsum('tef,efd->td', a, w_ed[g])
    return out.reshape(b, s, d)


def setup_inputs(seed: int = 0) -> dict:
    key = jax.random.key(seed)
    ks = jax.random.split(key, 24)
    f32 = jnp.float32

    def nrm(k, shape, std):
        return jax.random.normal(k, shape, f32) * std

    L, D = DEPTH, D_MODEL
    n_rel = 2 * MAX_REL + 1
    return {
        "x": nrm(ks[0], (BATCH, SEQ, D), 1.0),
        "c": nrm(ks[1], (BATCH, D), 1.0),
        "ada_w": nrm(ks[2], (L, D, 6 * D), 0.5 * D ** -0.5),
        "ada_b": nrm(ks[3], (L, 6 * D), 0.02),
        "norm_mix_g": 1.0 + nrm(ks[4], (L, D), 0.1),
        "norm_ffn_g": 1.0 + nrm(ks[5], (L, D), 0.1),
        "w_in": nrm(ks[6], (L, D, N_IN), D ** -0.5),
        "b_forget": 1.0 + nrm(ks[7], (L, H_A), 0.5),
        "rel_bias": nrm(ks[8], (L, H_B, n_rel), 0.5),
        "w_branch_a": nrm(ks[9], (L, W_A, D), W_A ** -0.5),
        "w_branch_b": nrm(ks[10], (L, W_B, D), W_B ** -0.5),
        "w_gate": nrm(ks[11], (L, D, 2 * D), D ** -0.5),
        "b_gate": nrm(ks[12], (L, 2 * D), 0.02),
        "w_out": nrm(ks[13], (L, D, D), D ** -0.5),
        "w_router_group": nrm(ks[14], (L, D, N_GROUPS), D ** -0.5),
        "b_router_group": nrm(ks[15], (L, N_GROUPS), 0.01),
        "w_router_expert": nrm(ks[16], (L, D, N_GROUPS * E_PER_GROUP), D ** -0.5),
        "b_router_expert": nrm(ks[17], (L, N_GROUPS * E_PER_GROUP), 0.01),
        "w_exp_gate": nrm(ks[18], (L, N_GROUPS, E_PER_GROUP, D, D_FF), D ** -0.5),
        "w_exp_up": nrm(ks[19], (L, N_GROUPS, E_PER_GROUP, D, D_FF), D ** -0.5),
        "w_exp_down": nrm(ks[20], (L, N_GROUPS, E_PER_GROUP, D_FF, D), D_FF ** -0.5),
        "final_norm_g": 1.0 + nrm(ks[21], (D,), 0.1),
    }


def reference(x, c, ada_w, ada_b, norm_mix_g, norm_ffn_g, w_in, b_forget, rel_bias,
              w_branch_a, w_branch_b, w_gate, b_gate, w_out,
              w_router_group, b_router_group, w_router_expert, b_router_expert,
              w_exp_gate, w_exp_up, w_exp_down, final_norm_g):
    c_act = jax.nn.silu(c)
    for l in range(DEPTH):
        mod = c_act @ ada_w[l] + ada_b[l]
        sh_m, sc_m, gt_m, sh_f, sc_f, gt_f = jnp.split(mod, 6, axis=-1)
        h = modulate(rmsnorm(x, norm_mix_g[l]), sh_m, sc_m)
        y = hybrid_mixer(h, w_in[l], b_forget[l], rel_bias[l], w_branch_a[l], w_branch_b[l],
                         w_gate[l], b_gate[l], w_out[l])
        x = x + gt_m[:, None, :] * y
        h = modulate(rmsnorm(x, norm_ffn_g[l]), sh_f, sc_f)
        y = hierarchical_moe(h, w_router_group[l], b_router_group[l], w_router_expert[l],
                             b_router_expert[l], w_exp_gate[l], w_exp_up[l], w_exp_down[l])
        x = x + gt_f[:, None, :] * y
    return rmsnorm(x, final_norm_g)
```

```python
import contextlib
import numpy as np
import ml_dtypes
import concourse.bass as bass
import concourse.mybir as mybir
from concourse.bass_utils import run_bass_kernel_spmd

F32 = mybir.dt.float32
BF16 = mybir.dt.bfloat16
AF = mybir.ActivationFunctionType
ALU = mybir.AluOpType
AX = mybir.AxisListType

D = 4096
T = 1024
NEG = -30000.0
SCALE = 128 ** -0.5
EPS = 1e-6
PAIRS = [[0, 1], [2, 3], [4, 5], [6, 7]]
ALL8 = [list(range(8))]
SHARD_W = True
WSHARD = {"w_in": 1, "w_gate": 1, "w_branch_a": 1, "w_branch_b": 1, "w_out": 1,
          "w_exp_gate": 2, "w_exp_up": 2, "w_exp_down": 2}


class Op:
    __slots__ = ("eng", "fn", "deps", "is_dma", "sem", "val", "need", "tag")

    def __init__(self, eng, fn, is_dma, tag=""):
        self.eng = eng
        self.fn = fn
        self.deps = []
        self.is_dma = is_dma
        self.sem = None
        self.val = 0
        self.need = False
        self.tag = tag


class Prog:
    ENG = ("pe", "act", "dve", "pool", "sp")

    def __init__(self, nc):
        self.nc = nc
        self.st = contextlib.ExitStack()
        self.ops = {e: [] for e in self.ENG}
        self.lastw = {}
        self.readers = {}
        self.n_dma_sems = {"sp": 24, "act": 4, "pool": 24}
        self.dma_hist = {"sp": [], "act": [], "pool": []}
        self.pending = {e: [] for e in self.ENG}
        self.phase = None

    def sb(self, name, shape, dt, persistent=False):
        st = self.st if (persistent or self.phase is None) else self.phase
        self.uid = getattr(self, "uid", 0) + 1
        return st.enter_context(self.nc.sbuf_tensor(f"{name}_{self.uid}", list(shape), dt))

    def ps(self, name, shape, dt):
        return self.st.enter_context(self.nc.psum_tensor(name, list(shape), dt))

    def begin_phase(self):
        assert self.phase is None
        self.phase = contextlib.ExitStack()

    def end_phase(self):
        self.barrier()
        self.phase.close()
        self.phase = None
        self.lastw = {}
        self.readers = {}

    def barrier(self):
        deps = []
        for e in ("pe", "act", "dve", "pool"):
            for op in reversed(self.ops[e]):
                if not op.is_dma:
                    deps.append(op)
                    break
        for q in ("sp", "act", "pool"):
            n = self.n_dma_sems[q]
            deps.extend(self.dma_hist[q][-n:])
        for e in self.ENG:
            self.pending[e] = list(deps)

    def _add(self, eng, fn, reads, writes, is_dma=False, tag=""):
        op = Op(eng, fn, is_dma, tag)
        deps = []
        lw = self.lastw
        rd = self.readers
        for k in reads:
            w = lw.get(k)
            if w is not None:
                deps.append(w)
        for k in writes:
            w = lw.get(k)
            if w is not None:
                deps.append(w)
            r = rd.get(k)
            if r:
                deps.extend(r[0].values())
                deps.extend(r[1])
        if self.pending[eng]:
            deps.extend(self.pending[eng])
            self.pending[eng] = []
        if is_dma:
            hist = self.dma_hist[eng]
            n = self.n_dma_sems[eng]
            if len(hist) >= n:
                deps.append(hist[len(hist) - n])
            hist.append(op)
        seen = set()
        for d in deps:
            if d is op or id(d) in seen:
                continue
            seen.add(id(d))
            if d.eng == "pe" and eng == "pe" and not is_dma and not d.is_dma:
                continue
            op.deps.append(d)
            d.need = True
        for k in reads:
            r = rd.get(k)
            if r is None:
                r = rd[k] = ({}, [])
            if is_dma:
                r[1].append(op)
            else:
                r[0][eng] = op
        for k in writes:
            lw[k] = op
            rd[k] = ({}, [])
        self.ops[eng].append(op)
        return op

    def pe(self, fn, reads, writes, tag=""):
        return self._add("pe", fn, reads, writes, tag=tag)

    def act(self, fn, reads, writes, tag=""):
        return self._add("act", fn, reads, writes, tag=tag)

    def dve(self, fn, reads, writes, tag=""):
        return self._add("dve", fn, reads, writes, tag=tag)

    def pool(self, fn, reads, writes, tag=""):
        return self._add("pool", fn, reads, writes, tag=tag)

    def dma(self, q, out, in_, reads, writes, tag="", **kw):
        def fn(e):
            return e.dma_start(out=out, in_=in_, **kw)
        return self._add(q, fn, reads, writes, is_dma=True, tag=tag)

    def emit(self, final_wait_ops=()):
        nc = self.nc
        st = self.st
        eng_sem = {e: st.enter_context(nc.semaphore("s_" + e)) for e in ("pe", "act", "dve", "pool")}
        dma_sems = {
            q: [st.enter_context(nc.semaphore(f"d_{q}{i}")) for i in range(n)]
            for q, n in self.n_dma_sems.items()
        }
        for q in ("sp", "act", "pool"):
            n = self.n_dma_sems[q]
            for i, op in enumerate(self.dma_hist[q]):
                op.sem = dma_sems[q][i % n]
                op.val = 16 * (i // n + 1)
        for e in ("pe", "act", "dve", "pool"):
            c = 0
            for op in self.ops[e]:
                if op.is_dma:
                    continue
                if op.need:
                    c += 1
                    op.sem = eng_sem[e]
                    op.val = c

        def replay(ename, handle):
            waited = {}
            for op in self.ops[ename]:
                for d in op.deps:
                    key = id(d.sem)
                    if waited.get(key, 0) >= d.val:
                        continue
                    handle.wait_ge(d.sem, d.val)
                    waited[key] = d.val
                ins = op.fn(handle)
                if op.is_dma:
                    ins.then_inc(op.sem, 16)
                elif op.need:
                    ins.then_inc(op.sem, 1)
            return waited

        def finals(h, w):
            for op in final_wait_ops:
                if w.get(id(op.sem), 0) < op.val:
                    h.wait_ge(op.sem, op.val)
                    w[id(op.sem)] = op.val

        with nc.Block() as block:
            @block.tensor
            def _(h):
                replay("pe", h)

            @block.vector
            def _(h):
                replay("dve", h)

            @block.scalar
            def _(h):
                replay("act", h)

            @block.gpsimd
            def _(h):
                finals(h, replay("pool", h))

            @block.sync
            def _(h):
                finals(h, replay("sp", h))
        st.close()


def build(n_layers=2, stop=None, dbg=None):
    nc = bass.Bass("TRN2", target_bir_lowering=False)
    P = Prog(nc)

    def din(name, shape, dt=F32):
        return nc.dram_tensor(name, list(shape), dt, kind="ExternalInput").ap()

    def dscr(name, shape, dt):
        return nc.dram_tensor(name, list(shape), dt).ap()

    L = 2
    SHAPES = {
        "xT": ([D, T], F32), "cT_all": ([128, 128], F32), "ada_w_s": ([L, D, 3072], F32),
        "ada_bT": ([L, 128, 24], F32), "bsel": ([128, 4], F32), "pen": ([128, 1], F32),
        "gmixT": ([L, 128, 32], F32), "gffnT": ([L, 128, 32], F32), "gfinT": ([128, 32], F32),
        "w_in": ([L, D, 12304], F32), "bf_t": ([L, 128, 16], F32), "relx": ([L, 16, 768], F32),
        "w_branch_a": ([L, 2048, D], F32), "w_branch_b": ([L, 2048, D], F32), "w_gate": ([L, D, 2 * D], F32),
        "bgT": ([L, 128, 64], F32), "w_out": ([L, D, D], F32), "w_r": ([L, D, 36], F32),
        "br_t": ([L, 128, 36], F32), "w_exp_gate": ([L, 4, 8, D, 512], F32), "w_exp_up": ([L, 4, 8, D, 512], F32),
        "w_exp_down": ([L, 4, 8, 512, D], F32), "c_idb": ([128, 128], BF16), "c_idf": ([128, 128], F32),
        "c_tri": ([128, 128], F32), "relT": ([L, 16, 128, 640], F32), "c_cmask": ([128, 128], F32), "c_bmask": ([128, 640], F32),
    }
    _in_cache = {}

    class PW:
        def __init__(self, pieces, lead_shape):
            self.pieces = pieces
            self.lead_shape = lead_shape

        def __getitem__(self, key):
            n = len(self.lead_shape)
            idx = 0
            for i in range(n):
                idx = idx * self.lead_shape[i] + key[i]
            return self.pieces[idx][tuple(key[n:])]

    def I(name):
        if name not in _in_cache:
            shape, dt = SHAPES[name]
            if SHARD_W and name in WSHARD:
                nlead = WSHARD[name]
                lead = shape[:nlead]
                pshape = shape[nlead:]
                npieces = int(np.prod(lead))
                Rp = int(np.prod(pshape[:-1]))
                C = pshape[-1]
                part = din(name, [npieces, Rp // 8, C], dt)
                pieces = []
                dims = " ".join(f"a{i}" for i in range(len(pshape) - 1))
                for pi in range(npieces):
                    full = nc.dram_tensor(f"{name}_full{pi}", list(pshape), dt).ap()
                    full2 = full.rearrange(f"{dims} c -> ({dims}) c")
                    bounce = nc.dram_tensor(f"{name}_bnc{pi}", [Rp // 8, C], dt).ap()
                    bk = ("bnc", name, pi)
                    P.dma("sp", bounce, part[pi], [], [bk])
                    P._add("pool", lambda e, bounce=bounce, full2=full2: e.collective_compute(
                        "AllGather", ALU.bypass, replica_groups=ALL8, ins=[bounce], outs=[full2]), [bk], ["WALL"])
                    pieces.append(full)
                _in_cache[name] = PW(pieces, lead)
            else:
                _in_cache[name] = din(name, shape, dt)
        return _in_cache[name]

    outT = nc.dram_tensor("outT", [D, T], F32, kind="ExternalOutput").ap()
    dbg_aps = {}
    if dbg:
        for name, (shape, dt) in dbg.items():
            dbg_aps[name] = nc.dram_tensor(name, list(shape), dt, kind="ExternalOutput").ap()

    XD = dscr("XD", [32, 128, T], F32)
    ccin = [dscr(f"ccin{l}", [128, 96], F32) for l in range(L)]
    ccout = [dscr(f"ccout{l}", [1024, 96], F32) for l in range(L)]
    QTa = dscr("QTa", [16, 128, T], BF16)
    QTb = dscr("QTb", [16, 128, T], BF16)
    XBi = [dscr(f"XBi{l}", [8192, T], BF16) for l in range(L)]
    XBo = [[dscr(f"XBo{l}_{ch}", [2048, T], BF16) for ch in range(8)] for l in range(L)]
    LFi = [dscr(f"LFi{l}", [128, 128], F32) for l in range(L)]
    LFo = [dscr(f"LFo{l}", [256, 128], F32) for l in range(L)]
    GD = dscr("GD", [64, 128, T], BF16)
    AD = dscr("AD", [32, 4, 128, T], BF16)

    IDb = P.sb("IDb", [128, 128], BF16, True)
    IDf = P.sb("IDf", [128, 128], F32, True)
    TRI = P.sb("TRI", [128, 128], F32, True)
    CMASK = P.sb("CMASK", [128, 128], F32, True)
    BMASK = P.sb("BMASK", [128, 640], F32, True)
    ONESb = P.sb("ONESb", [128, 128], BF16, True)
    ONESf = P.sb("ONESf", [128, 128], F32, True)
    CST = P.sb("CST", [128, 4], F32, True)
    PEN = P.sb("PEN", [128, 1], F32, True)
    BSEL = P.sb("BSEL", [128, 4], F32, True)
    MOD = [P.sb(f"MOD{l}", [128, 192], F32, True) for l in range(L)]
    SC1 = [P.sb(f"SC1_{l}", [128, 32], F32, True) for l in range(L)]
    SC2 = [P.sb(f"SC2_{l}", [128, 32], F32, True) for l in range(L)]
    GFIN = P.sb("GFIN", [128, 32], F32, True)
    HT = P.sb("HT", [128, 32, T], BF16, True)
    LFO = P.sb("LFO", [128, 8, 16], F32, True)
    CUM = P.sb("CUM", [128, 16, 16], F32, True)
    NEGC = P.sb("NEGC", [128, 16, 16], F32, True)
    COMB = P.sb("COMB", [128, 8, 32], F32, True)

    PS = [P.ps(f"PS{i}", [128, 512], F32) for i in range(7)]
    PSB = P.ps("PSB", [128, 1024], BF16)

    def psk(i):
        return ("ps", i)

    final_ops = []

    def dump(name, src_ap, reads):
        if name in dbg_aps:
            final_ops.append(P.dma("sp", dbg_aps[name], src_ap, reads, [("dbg", name)]))

    def finish():
        if not final_ops:
            pass
        P.emit(final_ops)
        return nc

    P.dma("sp", IDb[:, :], I("c_idb"), [], ["IDb"])
    P.dma("sp", IDf[:, :], I("c_idf"), [], ["IDf"])
    P.dma("sp", TRI[:, :], I("c_tri"), [], ["TRI"])
    P.dma("sp", CMASK[:, :], I("c_cmask"), [], ["CMASK"])
    P.dma("sp", BMASK[:, :], I("c_bmask"), [], ["BMASK"])
    P.dma("sp", PEN[:, :], I("pen"), [], ["PEN"])
    P.dma("sp", BSEL[:, :], I("bsel"), [], ["BSEL"])
    P.dma("sp", GFIN[:, :], I("gfinT"), [], ["GFIN"])
    P.dve(lambda e: e.memset(ONESb[:, :], 1.0), [], ["ONESb"])
    P.dve(lambda e: e.memset(ONESf[:, :], 1.0), [], ["ONESf"])
    P.dve(lambda e: e.memset(CST[:, 0:1], 1.0), [], ["CST0"])
    P.dve(lambda e: e.memset(CST[:, 1:2], EPS), [], ["CST1"])
    P.dve(lambda e: e.memset(CST[:, 2:3], 0.0), [], ["CST2"])
    CSTK = ["CST0", "CST1", "CST2"]

    STOPS = ["mod", "gather", "w_in", "xchg", "attA", "attB", "branch", "wout", "moe_norm", "p1", "p2"]
    NEEDW = {"gather": list(WSHARD), "w_in": ["w_in", "w_gate"], "xchg": ["w_in", "w_gate"], "attA": ["w_in", "w_gate"],
             "attB": ["w_in", "w_gate"], "branch": ["w_in", "w_gate", "w_branch_a", "w_branch_b"],
             "wout": ["w_in", "w_gate", "w_branch_a", "w_branch_b", "w_out"],
             "moe_norm": ["w_in", "w_gate", "w_branch_a", "w_branch_b", "w_out"],
             "p1": ["w_in", "w_gate", "w_branch_a", "w_branch_b", "w_out", "w_exp_gate", "w_exp_up"],
             "p2": list(WSHARD), None: list(WSHARD), "mod": []}
    if SHARD_W:
        for nm in NEEDW[stop]:
            I(nm)
    P.begin_phase()
    WB = [P.sb(f"WBm{i}", [128, 32, 256], BF16) for i in range(3)]
    CALL = P.sb("CALL", [128, 128], F32)
    CACT = P.sb("CACT", [128, 128], BF16)
    MODS = P.sb("MODS", [128, 24, 4], F32)
    ABT = P.sb("ABT", [128, 24], F32)
    M4r = P.sb("M4", [128, 8, 96], F32)
    M4 = M4r[:, :, :].rearrange("p r (j b) -> p (r j) b", b=4)
    GM = P.sb("GM", [128, 32], F32)
    P.dma("sp", CALL[:, :], I("cT_all"), [], ["CALL"])
    P.act(lambda e: e.activation(out=CACT[:, :], in_=CALL[:, :], func=AF.Silu), ["CALL"], ["CACT"])
    nblk = 0
    for l in range(n_layers):
        for blk in range(12):
            wb = WB[nblk % 3]
            wk = ("WBm", nblk % 3)
            nblk += 1
            P.dma("pool", wb[:, :, :],
                  I("ada_w_s")[l, :, blk * 256:(blk + 1) * 256].rearrange("(k p) n -> p k n", p=128), [], [wk])
            for jj in range(2):
                j = blk * 2 + jj
                for k in range(32):
                    P.pe(lambda e, wb=wb, j=j, jj=jj, k=k: e.matmul(
                        PS[0][:, j * 4:(j + 1) * 4], wb[:, k, jj * 128:(jj + 1) * 128],
                        CACT[:, k * 4:(k + 1) * 4], start=(k == 0), stop=(k == 31)),
                        [wk, "CACT"], [psk(0)])
        P.dma("sp", ABT[:, :], I("ada_bT")[l], [], ["ABT"])
        psv = PS[0][:, 0:96].rearrange("p (j b) -> p j b", b=4)
        for b in range(4):
            P.dve(lambda e, b=b, psv=psv: e.tensor_tensor(out=MODS[:, :, b], in0=psv[:, :, b], in1=ABT[:, :],
                                                          op=ALU.add), [psk(0), "ABT"], ["MODS"])
        if stop == "mod_a":
            dump("d_mods", MODS[:, :, :], ["MODS"])
            P.end_phase()
            return finish()
        P.dma("sp", ccin[l], MODS[:, :, :].rearrange("p j b -> p (j b)"), ["MODS"], [("ccin", l)])
        P._add("pool", lambda e, l=l: e.collective_compute(
            "AllGather", ALU.bypass, replica_groups=ALL8, ins=[ccin[l]], outs=[ccout[l]]),
            [("ccin", l), "WALL"], [("ccout", l), "WALL"])
        P.dma("sp", M4r[:, :, :], ccout[l].rearrange("(r p) f -> p r f", p=128), [("ccout", l)], ["M4"])
        P.dve(lambda e, l=l: e.tensor_scalar(out=MOD[l][:, :], in0=M4[:, :, 0], scalar1=BSEL[:, 0:1], scalar2=None,
                                             op0=ALU.mult), ["M4", "BSEL"], [("MOD", l)])
        for b in range(1, 4):
            P.dve(lambda e, l=l, b=b: e.scalar_tensor_tensor(out=MOD[l][:, :], in0=M4[:, :, b], scalar=BSEL[:, b:b + 1],
                                                             in1=MOD[l][:, :], op0=ALU.mult, op1=ALU.add),
                  ["M4", "BSEL", ("MOD", l)], [("MOD", l)])
        P.dma("sp", GM[:, :], I("gmixT")[l], [], ["GM"])
        P.dve(lambda e, l=l: e.scalar_tensor_tensor(out=SC1[l][:, :], in0=MOD[l][:, 32:64], scalar=1.0, in1=GM[:, :],
                                                    op0=ALU.add, op1=ALU.mult), [("MOD", l), "GM"], [("SC1", l)])
        P.dma("sp", GM[:, :], I("gffnT")[l], [], ["GM"])
        P.dve(lambda e, l=l: e.scalar_tensor_tensor(out=SC2[l][:, :], in0=MOD[l][:, 128:160], scalar=1.0, in1=GM[:, :],
                                                    op0=ALU.add, op1=ALU.mult), [("MOD", l), "GM"], [("SC2", l)])
    if stop in ("mod", "gather"):
        dump("d_mod0", MOD[0][:, :], [("MOD", 0)])
        dump("d_sc1", SC1[0][:, :], [("SC1", 0)])
        P.end_phase()
        return finish()
    P.end_phase()

    def xsrc_in(k):
        return I("xT")[k * 128:(k + 1) * 128, :]

    def xsrc_xd(k):
        return XD[k]

    def norm_stage(xsrc, scale_t, shift_t, mode, l=0):
        P.begin_phase()
        XK = [P.sb(f"XK{i}", [128, T], F32) for i in range(3)]
        SQ = [P.sb(f"SQ{i}", [128, T], BF16) for i in range(2)]
        RSTD = P.sb("RSTD", [128, T], F32)
        TMP = [P.sb(f"TMP{i}", [128, T], F32) for i in range(2)]
        if mode == "moe":
            H2F = [P.sb(f"H2F{i}", [128, T], F32) for i in range(2)]
            WR = P.sb("WR", [128, 32, 36], F32)
            LT = P.sb("LT", [36, T], F32)
            LTM = P.sb("LTM", [128, 36], F32)
            BR = P.sb("BR", [128, 36], F32)
            RT = [P.sb(f"RT{i}", [128, 40], F32) for i in range(2)]
            for kq in range(4):
                P.dma("sp", WR[:, kq * 8:(kq + 1) * 8, :],
                      I("w_r")[l, kq * 1024:(kq + 1) * 1024, :].rearrange("(k p) n -> p k n", p=128),
                      ["WR"] if kq else [], ["WR"])
            P.dma("sp", BR[:, :], I("br_t")[l], [], ["BR"])
        if mode == "final":
            OT = [P.sb(f"OT{i}", [128, T], F32) for i in range(2)]
        for k in range(32):
            xk = XK[k % 3]
            xkk = ("XK", k % 3)
            sq = SQ[k % 2]
            sqk = ("SQ", k % 2)
            P.dma("sp", xk[:, :], xsrc(k), [("XD", k)], [xkk])
            P.act(lambda e, xk=xk, sq=sq: e.activation(out=sq[:, :], in_=xk[:, :], func=AF.Square), [xkk], [sqk])
            for tc in range(2):
                P.pe(lambda e, sq=sq, tc=tc, k=k: e.matmul(PS[tc][:, :], ONESb[:, :], sq[:, tc * 512:(tc + 1) * 512],
                                                           start=(k == 0), stop=(k == 31)),
                     [sqk, "ONESb"], [psk(tc)])
        for tc in range(2):
            P.act(lambda e, tc=tc: e.activation(out=RSTD[:, tc * 512:(tc + 1) * 512], in_=PS[tc][:, :], func=AF.Sqrt,
                                                bias=CST[:, 1:2], scale=1.0 / D), [psk(tc), "CST1"], [("RSTD", tc)])
            P.dve(lambda e, tc=tc: e.reciprocal(out=RSTD[:, tc * 512:(tc + 1) * 512],
                                                in_=RSTD[:, tc * 512:(tc + 1) * 512]), [("RSTD", tc)], [("RSTD", tc)])
        for k in range(32):
            xk = XK[k % 3]
            xkk = ("XK", k % 3)
            tm = TMP[k % 2]
            tmk = ("TMP", k % 2)
            P.dma("sp", xk[:, :], xsrc(k), [("XD", k)], [xkk])
            P.dve(lambda e, xk=xk, tm=tm: e.tensor_tensor(out=tm[:, :], in0=xk[:, :], in1=RSTD[:, :], op=ALU.mult),
                  [xkk, ("RSTD", 0), ("RSTD", 1)], [tmk])
            if mode == "final":
                ot = OT[k % 2]
                otk = ("OT", k % 2)
                P.act(lambda e, tm=tm, ot=ot, k=k: e.activation(out=ot[:, :], in_=tm[:, :], func=AF.Identity,
                                                                bias=CST[:, 2:3], scale=GFIN[:, k:k + 1]),
                      [tmk, "GFIN", "CST2"], [otk])
                final_ops.append(P.dma("sp", outT[k * 128:(k + 1) * 128, :], ot[:, :], [otk], [("outT", k)]))
                continue
            P.act(lambda e, tm=tm, k=k: e.activation(out=HT[:, k, :], in_=tm[:, :], func=AF.Identity,
                                                     bias=shift_t[:, k:k + 1], scale=scale_t[:, k:k + 1]),
                  [tmk, ("MOD", l), ("SC1", l), ("SC2", l)], [("HT", k)])
            if mode == "moe":
                hf = H2F[k % 2]
                hfk = ("H2F", k % 2)
                P.act(lambda e, tm=tm, hf=hf, k=k: e.activation(out=hf[:, :], in_=tm[:, :], func=AF.Identity,
                                                                bias=shift_t[:, k:k + 1], scale=scale_t[:, k:k + 1]),
                      [tmk, ("MOD", l), ("SC2", l)], [hfk])
                for tc in range(2):
                    P.pe(lambda e, hf=hf, tc=tc, k=k: e.matmul(PS[2 + tc][0:36, :], WR[:, k, :],
                                                               hf[:, tc * 512:(tc + 1) * 512],
                                                               start=(k == 0), stop=(k == 31)),
                         [hfk, "WR"], [psk(2 + tc)])
        if mode == "moe":
            for tc in range(2):
                P.act(lambda e, tc=tc: e.activation(out=LT[:, tc * 512:(tc + 1) * 512], in_=PS[2 + tc][0:36, :],
                                                    func=AF.Identity, bias=CST[0:36, 2:3], scale=1.0),
                      [psk(2 + tc), "CST2"], [("LT", tc)])
            for tt in range(8):
                pb = 4 + (tt % 2)
                rt = RT[tt % 2]
                rk = ("RT", tt % 2)
                P.pe(lambda e, tt=tt, pb=pb: e.matmul(PS[pb][:, 0:36], LT[:, tt * 128:(tt + 1) * 128], IDf[0:36, 0:36],
                                                      start=True, stop=True), [("LT", tt // 4), "IDf"], [psk(pb)])
                P.dve(lambda e, pb=pb: e.tensor_tensor(out=LTM[:, :], in0=PS[pb][:, 0:36], in1=BR[:, :], op=ALU.add),
                      [psk(pb), "BR"], ["LTM"])
                routing(tt, LTM, rt, rk)
        P.end_phase()

    def routing(tt, Lg, rt, rk):
        rd = ["LTM"]
        P.dve(lambda e: e.tensor_reduce(out=rt[:, 0:1], in_=Lg[:, 0:4], axis=AX.X, op=ALU.max), rd, [rk])
        P.dve(lambda e: e.tensor_scalar(out=rt[:, 1:2], in0=rt[:, 0:1], scalar1=-1.0, scalar2=None, op0=ALU.mult),
              [rk], [rk])
        P.dve(lambda e: e.tensor_scalar(out=rt[:, 4:8], in0=Lg[:, 0:4], scalar1=rt[:, 0:1], scalar2=None,
                                        op0=ALU.is_equal), rd + [rk], [rk])
        P.act(lambda e: e.activation(out=rt[:, 16:20], in_=Lg[:, 0:4], func=AF.Exp, bias=rt[:, 1:2], scale=1.0),
              rd + [rk], [rk])
        P.dve(lambda e: e.tensor_reduce(out=rt[:, 2:3], in_=rt[:, 16:20], axis=AX.X, op=ALU.add), [rk], [rk])
        P.dve(lambda e: e.reciprocal(out=rt[:, 3:4], in_=rt[:, 2:3]), [rk], [rk])
        P.dve(lambda e: e.tensor_scalar(out=rt[:, 8:16], in0=Lg[:, 4:12], scalar1=rt[:, 4:5], scalar2=None,
                                        op0=ALU.mult), rd + [rk], [rk])
        for g in range(1, 4):
            P.dve(lambda e, g=g: e.scalar_tensor_tensor(out=rt[:, 8:16], in0=Lg[:, 4 + 8 * g:12 + 8 * g],
                                                        scalar=rt[:, 4 + g:5 + g], in1=rt[:, 8:16],
                                                        op0=ALU.mult, op1=ALU.add), rd + [rk], [rk])
        P.dve(lambda e: e.tensor_reduce(out=rt[:, 32:33], in_=rt[:, 8:16], axis=AX.X, op=ALU.max), [rk], [rk])
        P.dve(lambda e: e.tensor_scalar(out=rt[:, 16:24], in0=rt[:, 8:16], scalar1=rt[:, 32:33], scalar2=None,
                                        op0=ALU.is_equal), [rk], [rk])
        P.dve(lambda e: e.scalar_tensor_tensor(out=rt[:, 24:32], in0=rt[:, 16:24], scalar=-1e30, in1=rt[:, 8:16],
                                               op0=ALU.mult, op1=ALU.add), [rk], [rk])
        P.dve(lambda e: e.tensor_reduce(out=rt[:, 33:34], in_=rt[:, 24:32], axis=AX.X, op=ALU.max), [rk], [rk])
        P.dve(lambda e: e.tensor_scalar(out=rt[:, 24:32], in0=rt[:, 24:32], scalar1=rt[:, 33:34], scalar2=None,
                                        op0=ALU.is_equal), [rk], [rk])
        P.dve(lambda e: e.tensor_tensor(out=rt[:, 34:35], in0=rt[:, 33:34], in1=rt[:, 32:33], op=ALU.subtract),
              [rk], [rk])
        P.act(lambda e: e.activation(out=rt[:, 34:35], in_=rt[:, 34:35], func=AF.Exp, bias=CST[:, 2:3], scale=1.0),
              [rk, "CST2"], [rk])
        P.dve(lambda e: e.tensor_scalar(out=rt[:, 35:36], in0=rt[:, 34:35], scalar1=1.0, scalar2=None, op0=ALU.add),
              [rk], [rk])
        P.dve(lambda e: e.reciprocal(out=rt[:, 35:36], in_=rt[:, 35:36]), [rk], [rk])
        P.dve(lambda e: e.tensor_tensor(out=rt[:, 36:37], in0=rt[:, 34:35], in1=rt[:, 35:36], op=ALU.mult),
              [rk], [rk])
        P.dve(lambda e: e.tensor_tensor(out=rt[:, 37:38], in0=rt[:, 35:36], in1=rt[:, 3:4], op=ALU.mult), [rk], [rk])
        P.dve(lambda e: e.tensor_tensor(out=rt[:, 38:39], in0=rt[:, 36:37], in1=rt[:, 3:4], op=ALU.mult), [rk], [rk])
        P.dve(lambda e: e.tensor_scalar(out=rt[:, 16:24], in0=rt[:, 16:24], scalar1=rt[:, 37:38], scalar2=None,
                                        op0=ALU.mult), [rk], [rk])
        P.dve(lambda e: e.scalar_tensor_tensor(out=rt[:, 16:24], in0=rt[:, 24:32], scalar=rt[:, 38:39],
                                               in1=rt[:, 16:24], op0=ALU.mult, op1=ALU.add), [rk], [rk])
        for g in range(4):
            P.dve(lambda e, g=g: e.tensor_scalar(out=COMB[:, tt, 8 * g:8 * g + 8], in0=rt[:, 16:24],
                                                 scalar1=rt[:, 4 + g:5 + g], scalar2=None, op0=ALU.mult),
                  [rk], [("COMB", tt)])

    def stream_cols(wsrc_fn, nblocks, consume, WB, wctr):
        for i in range(nblocks):
            wb = WB[wctr[0] % 3]
            wk = ("WB", wctr[0] % 3)
            wctr[0] += 1
            for dst_fn, src in wsrc_fn(i):
                P.dma("pool", dst_fn(wb), src, [], [wk])
            consume(i, wb, wk)

    evc = [0]

    def evac(ps_ap, pskey, out_ap, outkey, extra_reads=()):
        evc[0] += 1
        if evc[0] % 2:
            P.act(lambda e: e.activation(out=out_ap, in_=ps_ap, func=AF.Identity, bias=CST[:, 2:3], scale=1.0),
                  [pskey, "CST2"] + list(extra_reads), [outkey])
        else:
            P.dve(lambda e: e.tensor_copy(out=out_ap, in_=ps_ap), [pskey] + list(extra_reads), [outkey])

    def layer(l):
        xsrc = xsrc_in if l == 0 else xsrc_xd
        norm_stage(xsrc, SC1[l], MOD[l][:, 0:32], "h", l)
        P.begin_phase()
        WB = [P.sb(f"WB{i}", [128, 32, 256], BF16) for i in range(3)]
        EV = [P.sb(f"EV{i}", [128, 512], BF16) for i in range(4)]
        WF = P.sb("WF", [128, 32, 16], BF16)
        BFT = P.sb("BFT", [128, 16], F32)
        LX = [P.sb(f"LX{i}", [128, 16], F32) for i in range(2)]
        LA = [P.sb(f"LA{i}", [128, 16], F32) for i in range(2)]
        LM = [P.sb(f"LM{i}", [128, 16], F32) for i in range(2)]
        BGT = P.sb("BGT", [128, 64], F32)
        P.dma("sp", BFT[:, :], I("bf_t")[l], [], ["BFT"])
        P.dma("sp", BGT[:, :], I("bgT")[l], [], ["BGT"])
        wctr = [0]
        psc = [0]
        evi = [0]
        uq = [0]

        def ukey():
            uq[0] += 1
            return ("u", uq[0])

        VA_view = XBi[l][4096:6144, :].rearrange("(t a) c -> t (a c)", a=2)
        VB_view = XBi[l][6144:8192, :].rearrange("(t a) c -> t (a c)", a=2)
        w_in_l = I("w_in")
        segs = [("qa", 0), ("ka", 2048), ("va", 4096), ("qb", 6160), ("kb", 8208), ("vb", 10256)]
        for seg, c0 in segs:
            for bi in range(8):
                wb = WB[wctr[0] % 3]
                wk = ("WB", wctr[0] % 3)
                wctr[0] += 1
                P.dma("pool", wb[:, :, :],
                      w_in_l[l, :, c0 + bi * 256:c0 + (bi + 1) * 256].rearrange("(k p) n -> p k n", p=128), [], [wk])
                if seg in ("qa", "ka", "qb", "kb"):
                    for hh in range(2):
                        h = bi * 2 + hh
                        for tc in range(2):
                            pb = psc[0] % 4
                            psc[0] += 1
                            for k in range(32):
                                P.pe(lambda e, wb=wb, hh=hh, tc=tc, k=k, pb=pb: e.matmul(
                                    PS[pb][:, :], wb[:, k, hh * 128:(hh + 1) * 128], HT[:, k, tc * 512:(tc + 1) * 512],
                                    start=(k == 0), stop=(k == 31)), [wk, ("HT", k)], [psk(pb)])
                            ev = EV[evi[0] % 4]
                            evk = ("EV", evi[0] % 4)
                            evi[0] += 1
                            evac(PS[pb][:, :], psk(pb), ev[:, :], evk)
                            if seg == "qa":
                                dst = QTa[h][:, tc * 512:(tc + 1) * 512]
                            elif seg == "qb":
                                dst = QTb[h][:, tc * 512:(tc + 1) * 512]
                            elif seg == "ka":
                                dst = XBi[l][h * 128:(h + 1) * 128, tc * 512:(tc + 1) * 512]
                            else:
                                dst = XBi[l][2048 + h * 128:2048 + (h + 1) * 128, tc * 512:(tc + 1) * 512]
                            P.dma("sp", dst, ev[:, :], [evk], [ukey()])
                else:
                    vview = VA_view if seg == "va" else VB_view
                    for tt in range(8):
                        pb = psc[0] % 4
                        psc[0] += 1
                        for k in range(32):
                            P.pe(lambda e, wb=wb, tt=tt, k=k, pb=pb: e.matmul(
                                PS[pb][:, 0:256], HT[:, k, tt * 128:(tt + 1) * 128], wb[:, k, :],
                                start=(k == 0), stop=(k == 31)), [wk, ("HT", k)], [psk(pb)])
                        ev = EV[evi[0] % 4]
                        evk = ("EV", evi[0] % 4)
                        evi[0] += 1
                        evac(PS[pb][:, 0:256], psk(pb), ev[:, 0:256], evk)
                        P.dma("sp", vview[tt * 128:(tt + 1) * 128, bi * 256:(bi + 1) * 256], ev[:, 0:256],
                              [evk], [ukey()])
        for kq in range(4):
            P.dma("pool", WF[:, kq * 8:(kq + 1) * 8, :],
                  w_in_l[l, kq * 1024:(kq + 1) * 1024, 6144:6160].rearrange("(k p) n -> p k n", p=128),
                  ["WF"] if kq else [], ["WF"])
        for tt in range(8):
            pb = 4 + tt % 2
            i2 = tt % 2
            for k in range(32):
                P.pe(lambda e, tt=tt, k=k, pb=pb: e.matmul(PS[pb][:, 0:16], HT[:, k, tt * 128:(tt + 1) * 128], WF[:, k, :],
                                                          start=(k == 0), stop=(k == 31)), ["WF", ("HT", k)], [psk(pb)])
            lx, la, lm = LX[i2], LA[i2], LM[i2]
            P.dve(lambda e, pb=pb, lx=lx: e.tensor_tensor(out=lx[:, :], in0=PS[pb][:, 0:16], in1=BFT[:, :], op=ALU.add),
                  [psk(pb), "BFT"], [("LX", i2)])
            P.act(lambda e, lx=lx, la=la: e.activation(out=la[:, :], in_=lx[:, :], func=AF.Abs, bias=CST[:, 2:3], scale=1.0),
                  [("LX", i2), "CST2"], [("LA", i2)])
            P.act(lambda e, la=la: e.activation(out=la[:, :], in_=la[:, :], func=AF.Exp, bias=CST[:, 2:3], scale=-1.0),
                  [("LA", i2), "CST2"], [("LA", i2)])
            P.act(lambda e, la=la: e.activation(out=la[:, :], in_=la[:, :], func=AF.Ln, bias=CST[:, 0:1], scale=1.0),
                  [("LA", i2), "CST0"], [("LA", i2)])
            P.dve(lambda e, lx=lx, lm=lm: e.tensor_scalar(out=lm[:, :], in0=lx[:, :], scalar1=0.0, scalar2=None,
                                                          op0=ALU.min), [("LX", i2)], [("LM", i2)])
            P.dve(lambda e, lm=lm, la=la, tt=tt: e.tensor_tensor(out=LFO[:, tt, :], in0=lm[:, :], in1=la[:, :],
                                                                 op=ALU.subtract),
                  [("LM", i2), ("LA", i2)], [("LFO", tt)])
        P.dma("sp", LFi[l], LFO[:, :, :].rearrange("p a b -> p (a b)"), [("LFO", tt) for tt in range(8)], [ukey()])
        w_gate_l = I("w_gate")
        for gb in range(32):
            wb = WB[wctr[0] % 3]
            wk = ("WB", wctr[0] % 3)
            wctr[0] += 1
            P.dma("pool", wb[:, :, :], w_gate_l[l, :, gb * 256:(gb + 1) * 256].rearrange("(k p) n -> p k n", p=128),
                  [], [wk])
            for cc in range(2):
                j = gb * 2 + cc
                for tc in range(2):
                    pb = psc[0] % 4
                    psc[0] += 1
                    for k in range(32):
                        P.pe(lambda e, wb=wb, cc=cc, tc=tc, k=k, pb=pb: e.matmul(
                            PS[pb][:, :], wb[:, k, cc * 128:(cc + 1) * 128], HT[:, k, tc * 512:(tc + 1) * 512],
                            start=(k == 0), stop=(k == 31)), [wk, ("HT", k)], [psk(pb)])
                    ev = EV[evi[0] % 4]
                    evk = ("EV", evi[0] % 4)
                    evi[0] += 1
                    P.act(lambda e, pb=pb, ev=ev, j=j: e.activation(out=ev[:, :], in_=PS[pb][:, :], func=AF.Sigmoid,
                                                                   bias=BGT[:, j:j + 1], scale=1.0),
                          [psk(pb), "BGT"], [evk])
                    P.dma("sp", GD[j][:, tc * 512:(tc + 1) * 512], ev[:, :], [evk], [ukey()])
        P.end_phase()

        if stop == "w_in":
            P.begin_phase()
            DBG = P.sb("DBG", [128, 1024], F32)
            dump("d_out", LFO[:, :, :].rearrange("p a b -> p (a b)"), [])
            P.end_phase()
            return "stop"
        for ch in range(8):
            P._add("pool", lambda e, l=l, ch=ch: e.collective_compute(
                "AllGather", ALU.bypass, replica_groups=PAIRS, ins=[XBi[l][ch * 1024:(ch + 1) * 1024, :]],
                outs=[XBo[l][ch]]), [("XBo", l)], [("XBo", l)])
        P._add("pool", lambda e, l=l: e.collective_compute(
            "AllGather", ALU.bypass, replica_groups=PAIRS, ins=[LFi[l]], outs=[LFo[l]]), [("XBo", l)], [("LFo", l)])
        P.begin_phase()
        LFP = P.sb("LFP", [128, 8, 16], F32)
        P.dma("sp", LFP[:, :, :], LFo[l][0:128, :].rearrange("p (a b) -> p a b", b=16), [("LFo", l)], ["LFP"])

        def lfsrc(i):
            return (LFP[:, i, :], "LFP") if i < 8 else (LFO[:, i - 8, :], ("LFO", i - 8))
        for i in range(16):
            pb = i % 4
            for i2 in range(i + 1):
                src, sk = lfsrc(i2)
                lhs = TRI if i2 == i else ONESf
                P.pe(lambda e, pb=pb, lhs=lhs, src=src, i2=i2, i=i: e.matmul(PS[pb][:, 0:16], lhs[:, :], src,
                                                                            start=(i2 == 0), stop=(i2 == i)),
                     [sk, "TRI", "ONESf"], [psk(pb)])
            P.dve(lambda e, pb=pb, i=i: e.tensor_copy(out=CUM[:, i, :], in_=PS[pb][:, 0:16]), [psk(pb)], [("CUM", i)])
            if i < 8:
                P.dve(lambda e, pb=pb, i=i: e.tensor_scalar(out=NEGC[:, i, :], in0=PS[pb][:, 0:16], scalar1=-1.0,
                                                            scalar2=PEN[:, 0:1], op0=ALU.mult, op1=ALU.add),
                      [psk(pb), "PEN"], [("NEGC", i)])
            else:
                P.dve(lambda e, pb=pb, i=i: e.tensor_scalar(out=NEGC[:, i, :], in0=PS[pb][:, 0:16], scalar1=-1.0,
                                                            scalar2=None, op0=ALU.mult), [psk(pb)], [("NEGC", i)])
        P.end_phase()

        if stop == "xchg":
            P.begin_phase()
            DBG = P.sb("DBG", [128, 1024], F32)
            dump("d_out", CUM[:, :, :].rearrange("p a b -> p (a b)"), [])
            P.end_phase()
            return "stop"
        def finish_o(ob, ON, RINV, ctr, YT, h, qblk):
            i2 = ctr[0] % 2
            sl = (ctr[0] % 8) * 128
            ctr[0] += 1
            on, rv = ON[i2], RINV[i2]
            P.dve(lambda e: e.reciprocal(out=rv[:, :], in_=PS[ob][:, 128:129]), [psk(ob)], [("RINV", i2)])
            P.dve(lambda e: e.tensor_scalar(out=on[:, :], in0=PS[ob][:, 0:128], scalar1=rv[:, 0:1], scalar2=None,
                                            op0=ALU.mult), [psk(ob), ("RINV", i2)], [("ON", i2)])
            P.pe(lambda e: e.transpose(PSB[:, sl:sl + 128], on[:, :], IDb[:, :]), [("ON", i2), "IDb"], ["psb"])
            P.act(lambda e: e.activation(out=YT[:, h, qblk * 128:(qblk + 1) * 128], in_=PSB[:, sl:sl + 128],
                                         func=AF.Identity, bias=CST[:, 2:3], scale=1.0),
                  ["psb", "CST2"], [("YT", id(YT), h, qblk)])

        mid = contextlib.ExitStack()
        YTA = mid.enter_context(nc.sbuf_tensor(f"YTA{l}", [128, 16, T], BF16))
        YTB = mid.enter_context(nc.sbuf_tensor(f"YTB{l}", [128, 16, T], BF16))
        P.begin_phase()
        QT = [P.sb(f"QT{i}", [128, T], BF16) for i in range(2)]
        KT = [P.sb(f"KT{i}", [128, 2048], BF16) for i in range(2)]
        VA = [P.sb(f"VA{i}", [128, 16, 129], BF16) for i in range(2)]
        CQB = [P.sb(f"CQB{i}", [128, T], F32) for i in range(2)]
        DG = [P.sb(f"DG{i}", [128, 128], F32) for i in range(2)]
        TMPA = [P.sb(f"TMPA{i}", [128, 512], F32) for i in range(3)]
        PT = [P.sb(f"PT{i}", [128, 512], BF16) for i in range(3)]
        ON = [P.sb(f"ON{i}", [128, 128], BF16) for i in range(2)]
        RINV = [P.sb(f"RINV{i}", [128, 1], F32) for i in range(2)]
        for i2 in range(2):
            P.dve(lambda e, i2=i2: e.memset(VA[i2][:, :, 128:129], 1.0), [], [("VAone", i2)])
        pVa_c = [XBo[l][4 + j][0:1024, :].rearrange("(t a) c -> t (a c)", a=2) for j in range(2)]
        oVa = XBi[l][4096:6144, :].rearrange("(t a) c -> t (a c)", a=2)
        sctr = [0]
        tctr = [0]
        octr = [0]
        for h in range(16):
            b2 = h % 2
            P.dma("sp", QT[b2][:, :], QTa[h], [], [("QT", b2)])
            P.dma("sp", KT[b2][:, 0:1024], XBo[l][h // 8][(h % 8) * 128:(h % 8 + 1) * 128, :], [("XBo", l)], [("KT", b2)])
            P.dma("sp", KT[b2][:, 1024:2048], XBi[l][h * 128:(h + 1) * 128, :], [("KT", b2)], [("KT", b2)])
            P.dma("sp", VA[b2][:, 0:4, 0:128], pVa_c[0][:, h * 128:(h + 1) * 128].rearrange("(i p) c -> p i c", p=128),
                  [("XBo", l), ("VAone", b2)], [("VA", b2)])
            P.dma("sp", VA[b2][:, 4:8, 0:128], pVa_c[1][:, h * 128:(h + 1) * 128].rearrange("(i p) c -> p i c", p=128),
                  [("VA", b2)], [("VA", b2)])
            P.dma("sp", VA[b2][:, 8:16, 0:128], oVa[:, h * 128:(h + 1) * 128].rearrange("(i p) c -> p i c", p=128),
                  [("VA", b2)], [("VA", b2)])
            for half in range(2):
                for t4 in range(4):
                    tt = half * 4 + t4
                    dg = DG[tt % 2]
                    P.dve(lambda e, dg=dg, tt=tt, h=h: e.tensor_scalar(out=dg[:, :], in0=IDf[:, :],
                                                                      scalar1=CUM[:, 8 + tt, h:h + 1], scalar2=None,
                                                                      op0=ALU.mult), ["IDf"], [("DG", tt % 2)])
                    P.pe(lambda e, dg=dg, t4=t4: e.matmul(PS[6][:, t4 * 128:(t4 + 1) * 128], ONESf[:, :], dg[:, :],
                                                          start=True, stop=True), [("DG", tt % 2), "ONESf"], [psk(6)])
                P.act(lambda e, b2=b2, half=half: e.activation(out=CQB[b2][:, half * 512:(half + 1) * 512],
                                                               in_=PS[6][:, :], func=AF.Identity, bias=CST[:, 2:3],
                                                               scale=1.0), [psk(6), "CST2"], [("CQB", b2, half)])
            for j in range(2):
                blocks = [(i, 512 * j) for i in range(8)] + [(8 + kb, max(512 * j, 128 * kb)) for kb in range(4 * j + 4)]
                for (i, qs) in blocks:
                    N = 512 * j + 512 - qs
                    sbk = sctr[0] % 2
                    sctr[0] += 1
                    ti = tctr[0] % 3
                    tctr[0] += 1
                    tm, pt = TMPA[ti], PT[ti]
                    P.pe(lambda e, sbk=sbk, N=N, b2=b2, i=i, qs=qs: e.matmul(
                        PS[sbk][:, 0:N], KT[b2][:, i * 128:(i + 1) * 128], QT[b2][:, qs:qs + N], start=True, stop=True),
                        [("KT", b2), ("QT", b2)], [psk(sbk)])
                    P.dve(lambda e, sbk=sbk, N=N, tm=tm, b2=b2, qs=qs: e.scalar_tensor_tensor(
                        out=tm[:, 0:N], in0=PS[sbk][:, 0:N], scalar=SCALE, in1=CQB[b2][:, qs:qs + N],
                        op0=ALU.mult, op1=ALU.add), [psk(sbk), ("CQB", b2, 0), ("CQB", b2, 1)], [("TMPA", ti)])
                    if i >= 8 and 128 * (i - 8) == qs:
                        P.dve(lambda e, tm=tm: e.tensor_tensor(out=tm[:, 0:128], in0=tm[:, 0:128], in1=CMASK[:, :],
                                                               op=ALU.add), [("TMPA", ti), "CMASK"], [("TMPA", ti)])
                    P.act(lambda e, tm=tm, pt=pt, N=N, i=i, h=h: e.activation(
                        out=pt[:, 0:N], in_=tm[:, 0:N], func=AF.Exp, bias=NEGC[:, i, h:h + 1], scale=1.0),
                        [("TMPA", ti)], [("PT", ti)])
                    for s in range(N // 128):
                        qblk = (qs + s * 128) // 128
                        ob = 2 + (qblk % 4)
                        P.pe(lambda e, pt=pt, s=s, ob=ob, b2=b2, i=i, qblk=qblk: e.matmul(
                            PS[ob][:, 0:129], pt[:, s * 128:(s + 1) * 128], VA[b2][:, i, :],
                            start=(i == 0), stop=(i == 8 + qblk)), [("PT", ti), ("VA", b2)], [psk(ob)])
                for s4 in range(4):
                    finish_o(2 + s4, ON, RINV, octr, YTA, h, 4 * j + s4)
        P.end_phase()

        if stop == "attA":
            P.begin_phase()
            DBG = P.sb("DBG", [128, 1024], F32)
            dump("d_out", YTA[:, 0, :], [])
            P.end_phase()
            mid.close()
            return "stop"
        P.begin_phase()
        QB = [P.sb(f"QB{i}", [128, T], BF16) for i in range(2)]
        KB = [P.sb(f"KB{i}", [128, 1536], BF16) for i in range(2)]
        VB = [P.sb(f"VB{i}", [128, 12, 129], BF16) for i in range(2)]
        BMr = [P.sb(f"BMr{i}", [128, 640], F32) for i in range(2)]
        BMm = [P.sb(f"BMm{i}", [128, 640], F32) for i in range(2)]
        BMP = [P.sb(f"BMP{i}", [128, 640], F32) for i in range(2)]
        TMPB = [P.sb(f"TMPB{i}", [128, 640], F32) for i in range(2)]
        PTB = [P.sb(f"PTB{i}", [128, 640], BF16) for i in range(2)]
        ON = [P.sb(f"ONb{i}", [128, 128], BF16) for i in range(2)]
        RINV = [P.sb(f"RINVb{i}", [128, 1], F32) for i in range(2)]
        for i2 in range(2):
            P.dve(lambda e, i2=i2: e.memset(VB[i2][:, :, 128:129], 1.0), [], [("VBone", i2)])
        pVb = XBo[l][7][0:1024, :].rearrange("(t a) c -> t (a c)", a=2)
        oVb = XBi[l][6144:8192, :].rearrange("(t a) c -> t (a c)", a=2)
        relT = I("relT")
        mctr = [0]
        for h in range(16):
            b2 = h % 2
            P.dma("sp", QB[b2][:, :], QTb[h], [], [("QB", b2)])
            P.dma("sp", KB[b2][:, 0:512], XBo[l][2 + h // 8][(h % 8) * 128:(h % 8 + 1) * 128, 512:1024], [], [("KB", b2)])
            P.dma("sp", KB[b2][:, 512:1536], XBi[l][2048 + h * 128:2048 + (h + 1) * 128, :], [("KB", b2)], [("KB", b2)])
            P.dma("sp", VB[b2][:, 0:4, 0:128],
                  pVb[:, h * 128:(h + 1) * 128].rearrange("(i p) c -> p i c", p=128), [("VBone", b2)], [("VB", b2)])
            P.dma("sp", VB[b2][:, 4:12, 0:128], oVb[:, h * 128:(h + 1) * 128].rearrange("(i p) c -> p i c", p=128),
                  [("VB", b2)], [("VB", b2)])
            P.dma("sp", BMr[b2][:, :], relT[l, h], [], [("BMr", b2)])
            P.dve(lambda e, b2=b2: e.tensor_tensor(out=BMm[b2][:, :], in0=BMr[b2][:, :], in1=BMASK[:, :], op=ALU.add),
                  [("BMr", b2), "BMASK"], [("BMm", b2)])
            P.dve(lambda e, b2=b2: e.tensor_scalar(out=BMP[b2][:, :], in0=BMm[b2][:, :], scalar1=PEN[:, 0:1],
                                                   scalar2=None, op0=ALU.add), [("BMm", b2), "PEN"], [("BMP", b2)])
            for m in range(8):
                mi = mctr[0] % 2
                mctr[0] += 1
                pa, pbk, ob = mi, 2 + mi, 4 + mi
                tm, pt = TMPB[mi], PTB[mi]
                for a in range(4):
                    P.pe(lambda e, pa=pa, a=a, m=m, b2=b2: e.matmul(
                        PS[pa][:, a * 128:(a + 1) * 128], KB[b2][:, (m + a) * 128:(m + a + 1) * 128],
                        QB[b2][:, m * 128:(m + 1) * 128], start=True, stop=True), [("KB", b2), ("QB", b2)], [psk(pa)])
                P.pe(lambda e, pbk=pbk, m=m, b2=b2: e.matmul(
                    PS[pbk][:, 0:128], KB[b2][:, (m + 4) * 128:(m + 5) * 128], QB[b2][:, m * 128:(m + 1) * 128],
                    start=True, stop=True), [("KB", b2), ("QB", b2)], [psk(pbk)])
                npv = max(0, 4 - m) * 128
                if npv > 0:
                    P.dve(lambda e, pa=pa, tm=tm, npv=npv, b2=b2: e.scalar_tensor_tensor(
                        out=tm[:, 0:npv], in0=PS[pa][:, 0:npv], scalar=SCALE, in1=BMP[b2][:, 0:npv],
                        op0=ALU.mult, op1=ALU.add), [psk(pa), ("BMP", b2)], [("TMPB", mi)])
                if npv < 512:
                    P.dve(lambda e, pa=pa, tm=tm, npv=npv, b2=b2: e.scalar_tensor_tensor(
                        out=tm[:, npv:512], in0=PS[pa][:, npv:512], scalar=SCALE, in1=BMm[b2][:, npv:512],
                        op0=ALU.mult, op1=ALU.add), [psk(pa), ("BMm", b2)], [("TMPB", mi)])
                P.dve(lambda e, pbk=pbk, tm=tm, b2=b2: e.scalar_tensor_tensor(
                    out=tm[:, 512:640], in0=PS[pbk][:, 0:128], scalar=SCALE, in1=BMm[b2][:, 512:640],
                    op0=ALU.mult, op1=ALU.add), [psk(pbk), ("BMm", b2)], [("TMPB", mi)])
                P.act(lambda e, tm=tm, pt=pt: e.activation(out=pt[:, :], in_=tm[:, :], func=AF.Exp, bias=CST[:, 2:3],
                                                           scale=1.0), [("TMPB", mi), "CST2"], [("PTB", mi)])
                for a in range(5):
                    P.pe(lambda e, ob=ob, pt=pt, a=a, m=m, b2=b2: e.matmul(
                        PS[ob][:, 0:129], pt[:, a * 128:(a + 1) * 128], VB[b2][:, m + a, :],
                        start=(a == 0), stop=(a == 4)), [("PTB", mi), ("VB", b2)], [psk(ob)])
                finish_o(ob, ON, RINV, octr, YTB, h, m)
        P.end_phase()

        if stop == "attB":
            P.begin_phase()
            DBG = P.sb("DBG", [128, 1024], F32)
            dump("d_out", YTB[:, 0, :], [])
            P.end_phase()
            mid.close()
            return "stop"
        P.begin_phase()
        WB = [P.sb(f"WBb{i}", [128, 32, 256], BF16) for i in range(2)]
        GA = [P.sb(f"GA{i}", [128, 2, T], BF16) for i in range(2)]
        GBt = [P.sb(f"GB{i}", [128, 2, T], BF16) for i in range(2)]
        T1 = [P.sb(f"T1{i}", [128, 512], F32) for i in range(2)]
        T2 = [P.sb(f"T2{i}", [128, 512], F32) for i in range(2)]
        wa_l, wb_l = I("w_branch_a"), I("w_branch_b")
        cnt = [0]
        for c2 in range(16):
            wb = WB[c2 % 2]
            wk = ("WB", c2 % 2)
            g2 = c2 % 2
            P.dma("pool", wb[:, 0:16, :], wa_l[l, :, c2 * 256:(c2 + 1) * 256].rearrange("(k p) n -> p k n", p=128),
                  [], [wk])
            P.dma("pool", wb[:, 16:32, :], wb_l[l, :, c2 * 256:(c2 + 1) * 256].rearrange("(k p) n -> p k n", p=128),
                  [wk], [wk])
            P.dma("sp", GA[g2][:, :, :], GD[c2 * 2:c2 * 2 + 2].rearrange("j p t -> p j t"), [], [("GA", g2)])
            P.dma("sp", GBt[g2][:, :, :], GD[32 + c2 * 2:32 + c2 * 2 + 2].rearrange("j p t -> p j t"), [], [("GB", g2)])
            for cc in range(2):
                for tc in range(2):
                    ci = cnt[0] % 2
                    cnt[0] += 1
                    pA, pB = ci, 2 + ci
                    for k in range(16):
                        P.pe(lambda e, wb=wb, cc=cc, tc=tc, k=k, pA=pA: e.matmul(
                            PS[pA][:, :], wb[:, k, cc * 128:(cc + 1) * 128], YTA[:, k, tc * 512:(tc + 1) * 512],
                            start=(k == 0), stop=(k == 15)), [wk], [psk(pA)])
                    for k in range(16):
                        P.pe(lambda e, wb=wb, cc=cc, tc=tc, k=k, pB=pB: e.matmul(
                            PS[pB][:, :], wb[:, 16 + k, cc * 128:(cc + 1) * 128], YTB[:, k, tc * 512:(tc + 1) * 512],
                            start=(k == 0), stop=(k == 15)), [wk], [psk(pB)])
                    t1, t2 = T1[ci], T2[ci]
                    P.dve(lambda e, t1=t1, pA=pA, g2=g2, cc=cc, tc=tc: e.tensor_tensor(
                        out=t1[:, :], in0=PS[pA][:, :], in1=GA[g2][:, cc, tc * 512:(tc + 1) * 512], op=ALU.mult),
                        [psk(pA), ("GA", g2)], [("T1", ci)])
                    P.dve(lambda e, t2=t2, pB=pB, g2=g2, cc=cc, tc=tc: e.tensor_tensor(
                        out=t2[:, :], in0=PS[pB][:, :], in1=GBt[g2][:, cc, tc * 512:(tc + 1) * 512], op=ALU.mult),
                        [psk(pB), ("GB", g2)], [("T2", ci)])
                    kc = c2 * 2 + cc
                    P.dve(lambda e, t1=t1, t2=t2, kc=kc, tc=tc: e.tensor_tensor(
                        out=HT[:, kc, tc * 512:(tc + 1) * 512], in0=t1[:, :], in1=t2[:, :], op=ALU.add),
                        [("T1", ci), ("T2", ci)], [("HT", kc)])
        P.end_phase()

        if stop == "branch":
            P.begin_phase()
            dump("d_out", HT[:, 0, :], [])
            P.end_phase()
            mid.close()
            return "stop"
        mid.close()

        P.begin_phase()
        WB = [P.sb(f"WBo{i}", [128, 32, 256], BF16) for i in range(3)]
        XK = [P.sb(f"XKo{i}", [128, T], F32) for i in range(2)]
        XN = [P.sb(f"XNo{i}", [128, T], F32) for i in range(2)]
        w_out_l = I("w_out")
        cnt = [0]
        for c2 in range(16):
            wb = WB[c2 % 3]
            wk = ("WB", c2 % 3)
            P.dma("pool", wb[:, :, :], w_out_l[l, :, c2 * 256:(c2 + 1) * 256].rearrange("(k p) n -> p k n", p=128),
                  [], [wk])
            for cc in range(2):
                kc = c2 * 2 + cc
                x2 = kc % 2
                P.dma("sp", XK[x2][:, :], xsrc(kc), [], [("XK", x2)])
                for tc in range(2):
                    pb = cnt[0] % 4
                    cnt[0] += 1
                    for k in range(32):
                        P.pe(lambda e, wb=wb, cc=cc, tc=tc, k=k, pb=pb: e.matmul(
                            PS[pb][:, :], wb[:, k, cc * 128:(cc + 1) * 128], HT[:, k, tc * 512:(tc + 1) * 512],
                            start=(k == 0), stop=(k == 31)), [wk], [psk(pb)])
                    P.dve(lambda e, pb=pb, x2=x2, kc=kc, tc=tc, l=l: e.scalar_tensor_tensor(
                        out=XN[x2][:, tc * 512:(tc + 1) * 512], in0=PS[pb][:, :], scalar=MOD[l][:, 64 + kc:65 + kc],
                        in1=XK[x2][:, tc * 512:(tc + 1) * 512], op0=ALU.mult, op1=ALU.add),
                        [psk(pb), ("XK", x2)], [("XN", x2, tc)])
                P.dma("sp", XD[kc], XN[x2][:, :], [("XN", x2, 0), ("XN", x2, 1)], [("XN", x2, 0), ("XN", x2, 1)])
        P.end_phase()

        if stop == "wout":
            P.begin_phase()
            DBG = P.sb("DBG", [128, 1024], F32)
            P.dma("sp", DBG[:, :], XD[0], [], ["DBG"])
            dump("d_out", DBG[:, :], ["DBG"])
            P.end_phase()
            return "stop"
        norm_stage(xsrc_xd, SC2[l], MOD[l][:, 96:128], "moe", l)

        if stop == "moe_norm":
            P.begin_phase()
            dump("d_out", COMB[:, :, :].rearrange("p a b -> p (a b)"), [])
            P.end_phase()
            return "stop"
        P.begin_phase()
        WB = [P.sb(f"WBe{i}", [128, 32, 256], BF16) for i in range(4)]
        CT = P.sb("CT", [32, T], F32)
        RS = [P.sb(f"RS{i}", [32, T], F32) for i in range(2)]
        CB = [P.sb(f"CB{i}", [128, T], F32) for i in range(2)]
        SG = [P.sb(f"SG{i}", [128, 512], F32) for i in range(2)]
        TU = [P.sb(f"TU{i}", [128, 512], F32) for i in range(2)]
        AT = [P.sb(f"AT{i}", [128, 512], BF16) for i in range(3)]
        for tt in range(8):
            pb = 4 + tt % 2
            P.pe(lambda e, tt=tt, pb=pb: e.matmul(PS[pb][0:32, 0:128], COMB[:, tt, :], IDf[:, :], start=True, stop=True),
                 [("COMB", tt), "IDf"], [psk(pb)])
            P.dve(lambda e, tt=tt, pb=pb: e.tensor_copy(out=CT[:, tt * 128:(tt + 1) * 128], in_=PS[pb][0:32, 0:128]),
                  [psk(pb)], ["CT"])
        weg, weu = I("w_exp_gate"), I("w_exp_up")
        wc = [0]
        cnt = [0]
        ai = [0]
        for ex in range(32):
            g, ee = ex // 8, ex % 8
            c2i = ex % 2
            rs, cb = RS[c2i], CB[c2i]
            P.dve(lambda e, rs=rs, ex=ex: e.tensor_scalar(out=rs[:, :], in0=CT[:, :], scalar1=IDf[0:32, ex:ex + 1],
                                                          scalar2=None, op0=ALU.mult), ["CT", "IDf"], [("RS", c2i)])
            for tc in range(2):
                P.pe(lambda e, rs=rs, tc=tc: e.matmul(PS[6][:, :], ONESf[0:32, :], rs[:, tc * 512:(tc + 1) * 512],
                                                      start=True, stop=True), [("RS", c2i), "ONESf"], [psk(6)])
                P.act(lambda e, cb=cb, tc=tc: e.activation(out=cb[:, tc * 512:(tc + 1) * 512], in_=PS[6][:, :],
                                                           func=AF.Identity, bias=CST[:, 2:3], scale=1.0),
                      [psk(6), "CST2"], [("CB", c2i, tc)])
            for fh in range(2):
                wg = WB[wc[0] % 4]
                wgk = ("WB", wc[0] % 4)
                wc[0] += 1
                wu = WB[wc[0] % 4]
                wuk = ("WB", wc[0] % 4)
                wc[0] += 1
                P.dma("pool", wg[:, :, :], weg[l, g, ee, :, fh * 256:(fh + 1) * 256].rearrange("(k p) n -> p k n", p=128),
                      [], [wgk])
                P.dma("pool", wu[:, :, :], weu[l, g, ee, :, fh * 256:(fh + 1) * 256].rearrange("(k p) n -> p k n", p=128),
                      [], [wuk])
                for fb in range(2):
                    fc = fh * 2 + fb
                    for tc in range(2):
                        ci = cnt[0] % 2
                        cnt[0] += 1
                        pG, pU = ci, 2 + ci
                        for k in range(32):
                            P.pe(lambda e, wg=wg, fb=fb, tc=tc, k=k, pG=pG: e.matmul(
                                PS[pG][:, :], wg[:, k, fb * 128:(fb + 1) * 128], HT[:, k, tc * 512:(tc + 1) * 512],
                                start=(k == 0), stop=(k == 31)), [wgk], [psk(pG)])
                        for k in range(32):
                            P.pe(lambda e, wu=wu, fb=fb, tc=tc, k=k, pU=pU: e.matmul(
                                PS[pU][:, :], wu[:, k, fb * 128:(fb + 1) * 128], HT[:, k, tc * 512:(tc + 1) * 512],
                                start=(k == 0), stop=(k == 31)), [wuk], [psk(pU)])
                        sg, tu = SG[ci], TU[ci]
                        a3 = ai[0] % 3
                        ai[0] += 1
                        at = AT[a3]
                        P.act(lambda e, sg=sg, pG=pG: e.activation(out=sg[:, :], in_=PS[pG][:, :], func=AF.Silu,
                                                                   bias=CST[:, 2:3], scale=1.0),
                              [psk(pG), "CST2"], [("SG", ci)])
                        P.dve(lambda e, tu=tu, sg=sg, pU=pU: e.tensor_tensor(out=tu[:, :], in0=PS[pU][:, :], in1=sg[:, :],
                                                                             op=ALU.mult),
                              [psk(pU), ("SG", ci)], [("TU", ci)])
                        P.dve(lambda e, tu=tu, at=at, cb=cb, tc=tc: e.tensor_tensor(
                            out=at[:, :], in0=tu[:, :], in1=cb[:, tc * 512:(tc + 1) * 512], op=ALU.mult),
                            [("TU", ci), ("CB", c2i, tc)], [("AT", a3)])
                        P.dma("sp", AD[ex, fc][:, tc * 512:(tc + 1) * 512], at[:, :], [("AT", a3)], [ukey()])
        P.end_phase()

        if stop == "p1":
            P.begin_phase()
            DBG = P.sb("DBG", [128, 1024], BF16)
            P.dma("sp", DBG[:, :], AD[0, 0], [], ["DBG"])
            dump("d_out", DBG[:, :], ["DBG"])
            P.end_phase()
            return "stop"
        P.begin_phase()
        WB = [P.sb(f"WBd{i}", [128, 32, 256], BF16) for i in range(3)]
        AE = [P.sb(f"AE{i}", [128, 4, T], BF16) for i in range(3)]
        XKd = [P.sb(f"XKd{i}", [128, T], F32) for i in range(2)]
        XNd = [P.sb(f"XNd{i}", [128, T], F32) for i in range(2)]
        wed = I("w_exp_down")
        wc = [0]
        ac = [0]
        for c2 in range(16):
            for g in range(4):
                wb = WB[wc[0] % 3]
                wk = ("WB", wc[0] % 3)
                wc[0] += 1
                P.dma("pool", wb[:, :, :],
                      wed[l, g, :, :, c2 * 256:(c2 + 1) * 256].rearrange("e (fc p) n -> p (e fc) n", p=128), [], [wk])
                for ee in range(8):
                    ex = g * 8 + ee
                    a3 = ac[0] % 3
                    ac[0] += 1
                    ae = AE[a3]
                    P.dma("sp", ae[:, :, :], AD[ex].rearrange("f p t -> p f t"), [], [("AE", a3)])
                    for fc in range(4):
                        for cc in range(2):
                            for tc in range(2):
                                pb = cc * 2 + tc
                                first = (ex == 0 and fc == 0)
                                last = (ex == 31 and fc == 3)
                                P.pe(lambda e, wb=wb, ee=ee, fc=fc, cc=cc, tc=tc, pb=pb, ae=ae, first=first, last=last:
                                     e.matmul(PS[pb][:, :], wb[:, ee * 4 + fc, cc * 128:(cc + 1) * 128],
                                              ae[:, fc, tc * 512:(tc + 1) * 512], start=first, stop=last),
                                     [wk, ("AE", a3)], [psk(pb)])
            for cc in range(2):
                kc = c2 * 2 + cc
                x2 = kc % 2
                P.dma("sp", XKd[x2][:, :], XD[kc], [], [("XK", x2)])
                for tc in range(2):
                    pb = cc * 2 + tc
                    P.dve(lambda e, pb=pb, x2=x2, kc=kc, tc=tc, l=l: e.scalar_tensor_tensor(
                        out=XNd[x2][:, tc * 512:(tc + 1) * 512], in0=PS[pb][:, :], scalar=MOD[l][:, 160 + kc:161 + kc],
                        in1=XKd[x2][:, tc * 512:(tc + 1) * 512], op0=ALU.mult, op1=ALU.add),
                        [psk(pb), ("XK", x2)], [("XN", x2, tc)])
                P.dma("sp", XD[kc], XNd[x2][:, :], [("XN", x2, 0), ("XN", x2, 1)], [("XN", x2, 0), ("XN", x2, 1)])
        P.end_phase()

    for l in range(n_layers):
        if layer(l) == "stop":
            return finish()

    norm_stage(xsrc_xd, None, None, "final", 0)
    return finish()


def _consts():
    idf = np.eye(128, dtype=np.float32)
    tri = (np.arange(128)[:, None] <= np.arange(128)[None, :]).astype(np.float32)
    cmask = np.where(np.arange(128)[None, :] >= np.arange(128)[:, None], 0.0, NEG).astype(np.float32)
    bmask = np.zeros((128, 5, 128), np.float32)
    kk = (np.arange(128) >= 64)[:, None]
    qq = (np.arange(128) >= 64)[None, :]
    bmask[:, 0, :] = np.where((~kk) & qq, NEG, 0.0)
    bmask[:, 4, :] = np.where(kk & (~qq), NEG, 0.0)
    return {
        "c_idb": idf.astype(ml_dtypes.bfloat16), "c_idf": idf, "c_tri": tri, "c_cmask": cmask,
        "c_bmask": bmask.reshape(128, 640),
    }


def _rel_idx():
    k = np.arange(128)[:, None, None]
    a = np.arange(5)[None, :, None]
    q = np.arange(128)[None, None, :]
    ext = q - k + 127 + 128 * (4 - a)
    return np.minimum(ext + 1, 256).reshape(128, 640)


_REL_IDX = _rel_idx()


def prep_inputs(inp):
    f = lambda a: np.ascontiguousarray(np.asarray(a, dtype=np.float32))
    x = np.asarray(inp["x"], np.float32)
    c = np.asarray(inp["c"], np.float32)
    L = 2
    vt = lambda v: f(v.reshape(-1, 128).T)
    shared = {
        "cT_all": f(c.reshape(4, 32, 128).transpose(2, 1, 0).reshape(128, 128)),
        "gmixT": f(np.stack([vt(inp["norm_mix_g"][l]) for l in range(L)])),
        "gffnT": f(np.stack([vt(inp["norm_ffn_g"][l]) for l in range(L)])),
        "gfinT": vt(np.asarray(inp["final_norm_g"])),
        "w_in": f(inp["w_in"]),
        "bf_t": f(np.broadcast_to(np.asarray(inp["b_forget"])[:, None, :], (L, 128, 16))),
        "relT": f(np.asarray(inp["rel_bias"])[:, :, _REL_IDX]),
        "w_branch_a": f(inp["w_branch_a"]), "w_branch_b": f(inp["w_branch_b"]),
        "w_gate": f(inp["w_gate"]),
        "bgT": f(np.stack([vt(np.asarray(inp["b_gate"])[l]) for l in range(L)])),
        "w_out": f(inp["w_out"]),
        "w_r": f(np.concatenate([np.asarray(inp["w_router_group"]), np.asarray(inp["w_router_expert"])], axis=2)),
        "br_t": f(np.broadcast_to(np.concatenate([np.asarray(inp["b_router_group"]),
                                                  np.asarray(inp["b_router_expert"])], axis=1)[:, None, :],
                                  (L, 128, 36))),
        "w_exp_gate": f(inp["w_exp_gate"]), "w_exp_up": f(inp["w_exp_up"]), "w_exp_down": f(inp["w_exp_down"]),
    }
    shared.update(_consts())
    ada_w = np.asarray(inp["ada_w"], np.float32)
    ada_b = np.asarray(inp["ada_b"], np.float32)
    maps = []
    flat = {}
    if SHARD_W:
        for nm, nlead in WSHARD.items():
            a = shared.pop(nm)
            npieces = int(np.prod(a.shape[:nlead]))
            flat[nm] = a.reshape(npieces, 8, -1, a.shape[-1])
    for cid in range(8):
        b, half = cid // 2, cid % 2
        m = dict(shared)
        if SHARD_W:
            for nm in WSHARD:
                m[nm] = np.ascontiguousarray(flat[nm][:, cid])
        m["xT"] = f(x[b, half * T:(half + 1) * T, :].T)
        m["ada_w_s"] = f(ada_w[:, :, cid * 3072:(cid + 1) * 3072])
        m["ada_bT"] = f(ada_b[:, cid * 3072:(cid + 1) * 3072].reshape(L, 24, 128).transpose(0, 2, 1))
        bs = np.zeros((128, 4), np.float32)
        bs[:, b] = 1.0
        m["bsel"] = bs
        m["pen"] = np.full((128, 1), 0.0 if half == 1 else NEG, np.float32)
        maps.append(m)
    return maps


def used_inputs(nc):
    names = set()
    for alloc in nc.allocations:
        if isinstance(alloc, mybir.MemoryLocationSet) and alloc.kind == "ExternalInput":
            names.add(alloc.memorylocations[0].name)
    return names


def kernel(**inputs):
    nc = build()
    used = used_inputs(nc)
    maps = [{k: v for k, v in m.items() if k in used} for m in prep_inputs(inputs)]
    res = run_bass_kernel_spmd(nc, maps, core_ids=list(range(8)))
    out = np.empty((4, 2048, D), np.float32)
    for cid in range(8):
        b, half = cid // 2, cid % 2
        out[b, half * T:(half + 1) * T, :] = res.results[cid]["outT"].T
    return out
```

```python
import contextlib
import numpy as np
import ml_dtypes
import concourse.bass as bass
import concourse.mybir as mybir
from concourse.bass_utils import run_bass_kernel_spmd

F32 = mybir.dt.float32
BF16 = mybir.dt.bfloat16
AF = mybir.ActivationFunctionType
ALU = mybir.AluOpType
AX = mybir.AxisListType

D = 4096
T = 1024
NEG = -30000.0
SCALE = 128 ** -0.5
EPS = 1e-6
PAIRS = [[0, 1], [2, 3], [4, 5], [6, 7]]
ALL8 = [list(range(8))]
SHARD_W = True
WSHARD = {"w_in": 1, "w_gate": 1, "w_branch_a": 1, "w_branch_b": 1, "w_out": 1,
          "w_exp_gate": 2, "w_exp_up": 2, "w_exp_down": 2}


class Op:
    __slots__ = ("eng", "fn", "deps", "is_dma", "sem", "val", "need", "tag")

    def __init__(self, eng, fn, is_dma, tag=""):
        self.eng = eng
        self.fn = fn
        self.deps = []
        self.is_dma = is_dma
        self.sem = None
        self.val = 0
        self.need = False
        self.tag = tag


class Prog:
    ENG = ("pe", "act", "dve", "pool", "sp")

    def __init__(self, nc):
        self.nc = nc
        self.st = contextlib.ExitStack()
        self.ops = {e: [] for e in self.ENG}
        self.lastw = {}
        self.readers = {}
        self.n_dma_sems = {"sp": 24, "act": 4, "pool": 24}
        self.dma_hist = {"sp": [], "act": [], "pool": []}
        self.pending = {e: [] for e in self.ENG}
        self.phase = None
        self.persist = {}
        self.wkey = None

    def sb(self, name, shape, dt, persistent=False):
        st = self.st if (persistent or self.phase is None) else self.phase
        self.uid = getattr(self, "uid", 0) + 1
        return st.enter_context(self.nc.sbuf_tensor(f"{name}_{self.uid}", list(shape), dt))

    def ps(self, name, shape, dt):
        return self.st.enter_context(self.nc.psum_tensor(name, list(shape), dt))

    def begin_phase(self):
        assert self.phase is None
        self.phase = contextlib.ExitStack()

    def end_phase(self):
        self.barrier()
        self.phase.close()
        self.phase = None
        self.lastw = {}
        self.readers = {}

    def barrier(self):
        deps = []
        for e in ("pe", "act", "dve", "pool"):
            for op in reversed(self.ops[e]):
                if not op.is_dma and op.tag != "gather":
                    deps.append(op)
                    break
        for q in ("sp", "act", "pool"):
            n = self.n_dma_sems[q]
            deps.extend(self.dma_hist[q][-n:])
        for e in self.ENG:
            self.pending[e] = list(deps)

    def _add(self, eng, fn, reads, writes, is_dma=False, tag=""):
        op = Op(eng, fn, is_dma, tag)
        deps = []
        lw = self.lastw
        rd = self.readers
        for k in reads:
            w = lw.get(k) or self.persist.get(k)
            if w is not None:
                deps.append(w)
        for k in writes:
            w = lw.get(k)
            if w is not None:
                deps.append(w)
            r = rd.get(k)
            if r:
                deps.extend(r[0].values())
                deps.extend(r[1])
        if self.pending[eng]:
            deps.extend(self.pending[eng])
            self.pending[eng] = []
        if is_dma:
            hist = self.dma_hist[eng]
            n = self.n_dma_sems[eng]
            if len(hist) >= n:
                deps.append(hist[len(hist) - n])
            hist.append(op)
        seen = set()
        for d in deps:
            if d is op or id(d) in seen:
                continue
            seen.add(id(d))
            if d.eng == "pe" and eng == "pe" and not is_dma and not d.is_dma:
                continue
            op.deps.append(d)
            d.need = True
        for k in reads:
            r = rd.get(k)
            if r is None:
                r = rd[k] = ({}, [])
            if is_dma:
                r[1].append(op)
            else:
                r[0][eng] = op
        for k in writes:
            lw[k] = op
            rd[k] = ({}, [])
        self.ops[eng].append(op)
        return op

    def pe(self, fn, reads, writes, tag=""):
        return self._add("pe", fn, reads, writes, tag=tag)

    def act(self, fn, reads, writes, tag=""):
        return self._add("act", fn, reads, writes, tag=tag)

    def dve(self, fn, reads, writes, tag=""):
        return self._add("dve", fn, reads, writes, tag=tag)

    def pool(self, fn, reads, writes, tag=""):
        return self._add("pool", fn, reads, writes, tag=tag)

    def dma(self, q, out, in_, reads, writes, tag="", **kw):
        if q == "pool" and self.wkey is not None:
            reads = list(reads) + [self.wkey]
            self.wkey = None

        def fn(e):
            return e.dma_start(out=out, in_=in_, **kw)
        return self._add(q, fn, reads, writes, is_dma=True, tag=tag)

    def emit(self, final_wait_ops=()):
        nc = self.nc
        st = self.st
        eng_sem = {e: st.enter_context(nc.semaphore("s_" + e)) for e in ("pe", "act", "dve", "pool")}
        dma_sems = {
            q: [st.enter_context(nc.semaphore(f"d_{q}{i}")) for i in range(n)]
            for q, n in self.n_dma_sems.items()
        }
        for q in ("sp", "act", "pool"):
            n = self.n_dma_sems[q]
            for i, op in enumerate(self.dma_hist[q]):
                op.sem = dma_sems[q][i % n]
                op.val = 16 * (i // n + 1)
        for e in ("pe", "act", "dve", "pool"):
            c = 0
            for op in self.ops[e]:
                if op.is_dma:
                    continue
                if op.tag == "gather":
                    op.sem = st.enter_context(nc.semaphore(f"g_{id(op)}"))
                    op.val = 1
                    op.need = True
                    continue
                if op.need:
                    c += 1
                    op.sem = eng_sem[e]
                    op.val = c

        def replay(ename, handle):
            waited = {}
            for op in self.ops[ename]:
                for d in op.deps:
                    key = id(d.sem)
                    if waited.get(key, 0) >= d.val:
                        continue
                    handle.wait_ge(d.sem, d.val)
                    waited[key] = d.val
                ins = op.fn(handle)
                if op.is_dma:
                    ins.then_inc(op.sem, 16)
                elif op.need:
                    ins.then_inc(op.sem, 1)
            return waited

        def finals(h, w):
            for op in final_wait_ops:
                if w.get(id(op.sem), 0) < op.val:
                    h.wait_ge(op.sem, op.val)
                    w[id(op.sem)] = op.val

        with nc.Block() as block:
            @block.tensor
            def _(h):
                replay("pe", h)

            @block.vector
            def _(h):
                replay("dve", h)

            @block.scalar
            def _(h):
                replay("act", h)

            @block.gpsimd
            def _(h):
                finals(h, replay("pool", h))

            @block.sync
            def _(h):
                finals(h, replay("sp", h))
        st.close()


def build(n_layers=2, stop=None, dbg=None):
    nc = bass.Bass("TRN2", target_bir_lowering=False)
    P = Prog(nc)

    def din(name, shape, dt=F32):
        return nc.dram_tensor(name, list(shape), dt, kind="ExternalInput").ap()

    def dscr(name, shape, dt):
        return nc.dram_tensor(name, list(shape), dt).ap()

    L = 2
    SHAPES = {
        "xT": ([D, T], F32), "cT_all": ([128, 128], F32), "ada_w_s": ([L, D, 3072], F32),
        "ada_bT": ([L, 128, 24], F32), "bsel": ([128, 4], F32), "pen": ([128, 1], F32),
        "gmixT": ([L, 128, 32], F32), "gffnT": ([L, 128, 32], F32), "gfinT": ([128, 32], F32),
        "w_in": ([L, D, 12304], F32), "bf_t": ([L, 128, 16], F32), "relx": ([L, 16, 768], F32),
        "w_branch_a": ([L, 2048, D], F32), "w_branch_b": ([L, 2048, D], F32), "w_gate": ([L, D, 2 * D], F32),
        "bgT": ([L, 128, 64], F32), "w_out": ([L, D, D], F32), "w_r": ([L, D, 36], F32),
        "br_t": ([L, 128, 36], F32), "w_exp_gate": ([L, 4, 8, D, 512], F32), "w_exp_up": ([L, 4, 8, D, 512], F32),
        "w_exp_down": ([L, 4, 8, 512, D], F32), "c_idb": ([128, 128], BF16), "c_idf": ([128, 128], F32),
        "c_tri": ([128, 128], F32), "relT": ([L, 16, 128, 640], F32), "c_cmask": ([128, 128], F32), "c_bmask": ([128, 640], F32),
    }
    _in_cache = {}

    class PW:
        def __init__(self, pieces, lead_shape, name):
            self.name = name
            self.pieces = pieces
            self.lead_shape = lead_shape

        def __getitem__(self, key):
            n = len(self.lead_shape)
            idx = 0
            for i in range(n):
                idx = idx * self.lead_shape[i] + key[i]
            P.wkey = ("W", self.name, idx)
            return self.pieces[idx][tuple(key[n:])]

    def I(name):
        if name not in _in_cache:
            shape, dt = SHAPES[name]
            if SHARD_W and name in WSHARD:
                nlead = WSHARD[name]
                lead = shape[:nlead]
                pshape = shape[nlead:]
                npieces = int(np.prod(lead))
                Rp = int(np.prod(pshape[:-1]))
                C = pshape[-1]
                part = din(name, [npieces, Rp // 8, C], dt)
                pieces = []
                dims = " ".join(f"a{i}" for i in range(len(pshape) - 1))
                for pi in range(npieces):
                    full = nc.dram_tensor(f"{name}_full{pi}", list(pshape), dt).ap()
                    full2 = full.rearrange(f"{dims} c -> ({dims}) c")
                    bounce = nc.dram_tensor(f"{name}_bnc{pi}", [Rp // 8, C], dt).ap()
                    bk = ("bnc", name, pi)
                    P.dma("sp", bounce, part[pi], [], [bk])
                    gop = P._add("pool", lambda e, bounce=bounce, full2=full2: e.collective_compute(
                        "AllGather", ALU.bypass, replica_groups=ALL8, ins=[bounce], outs=[full2]), [bk], [], tag="gather")
                    P.persist[("W", name, pi)] = gop
                    pieces.append(full)
                _in_cache[name] = PW(pieces, lead, name)
            else:
                _in_cache[name] = din(name, shape, dt)
        return _in_cache[name]

    outT = nc.dram_tensor("outT", [D, T], F32, kind="ExternalOutput").ap()
    dbg_aps = {}
    if dbg:
        for name, (shape, dt) in dbg.items():
            dbg_aps[name] = nc.dram_tensor(name, list(shape), dt, kind="ExternalOutput").ap()

    XD = dscr("XD", [32, 128, T], F32)
    ccin = [dscr(f"ccin{l}", [128, 96], F32) for l in range(L)]
    ccout = [dscr(f"ccout{l}", [1024, 96], F32) for l in range(L)]
    QTa = dscr("QTa", [16, 128, T], BF16)
    QTb = dscr("QTb", [16, 128, T], BF16)
    XBi = [dscr(f"XBi{l}", [8192, T], BF16) for l in range(L)]
    XBo = [[dscr(f"XBo{l}_{ch}", [2048, T], BF16) for ch in range(8)] for l in range(L)]
    LFi = [dscr(f"LFi{l}", [128, 128], F32) for l in range(L)]
    LFo = [dscr(f"LFo{l}", [256, 128], F32) for l in range(L)]
    GD = dscr("GD", [64, 128, T], BF16)
    AD = dscr("AD", [32, 4, 128, T], BF16)

    IDb = P.sb("IDb", [128, 128], BF16, True)
    IDf = P.sb("IDf", [128, 128], F32, True)
    TRI = P.sb("TRI", [128, 128], F32, True)
    CMASK = P.sb("CMASK", [128, 128], F32, True)
    BMASK = P.sb("BMASK", [128, 640], F32, True)
    ONESb = P.sb("ONESb", [128, 128], BF16, True)
    ONESf = P.sb("ONESf", [128, 128], F32, True)
    CST = P.sb("CST", [128, 4], F32, True)
    PEN = P.sb("PEN", [128, 1], F32, True)
    BSEL = P.sb("BSEL", [128, 4], F32, True)
    MOD = [P.sb(f"MOD{l}", [128, 192], F32, True) for l in range(L)]
    SC1 = [P.sb(f"SC1_{l}", [128, 32], F32, True) for l in range(L)]
    SC2 = [P.sb(f"SC2_{l}", [128, 32], F32, True) for l in range(L)]
    GFIN = P.sb("GFIN", [128, 32], F32, True)
    HT = P.sb("HT", [128, 32, T], BF16, True)
    LFO = P.sb("LFO", [128, 8, 16], F32, True)
    CUM = P.sb("CUM", [128, 16, 16], F32, True)
    NEGC = P.sb("NEGC", [128, 16, 16], F32, True)
    COMB = P.sb("COMB", [128, 8, 32], F32, True)

    PS = [P.ps(f"PS{i}", [128, 512], F32) for i in range(7)]
    PSB = P.ps("PSB", [128, 1024], BF16)

    def psk(i):
        return ("ps", i)

    final_ops = []

    def dump(name, src_ap, reads):
        if name in dbg_aps:
            final_ops.append(P.dma("sp", dbg_aps[name], src_ap, reads, [("dbg", name)]))

    def finish():
        if not final_ops:
            pass
        P.emit(final_ops)
        return nc

    P.dma("sp", IDb[:, :], I("c_idb"), [], ["IDb"])
    P.dma("sp", IDf[:, :], I("c_idf"), [], ["IDf"])
    P.dma("sp", TRI[:, :], I("c_tri"), [], ["TRI"])
    P.dma("sp", CMASK[:, :], I("c_cmask"), [], ["CMASK"])
    P.dma("sp", BMASK[:, :], I("c_bmask"), [], ["BMASK"])
    P.dma("sp", PEN[:, :], I("pen"), [], ["PEN"])
    P.dma("sp", BSEL[:, :], I("bsel"), [], ["BSEL"])
    P.dma("sp", GFIN[:, :], I("gfinT"), [], ["GFIN"])
    P.dve(lambda e: e.memset(ONESb[:, :], 1.0), [], ["ONESb"])
    P.dve(lambda e: e.memset(ONESf[:, :], 1.0), [], ["ONESf"])
    P.dve(lambda e: e.memset(CST[:, 0:1], 1.0), [], ["CST0"])
    P.dve(lambda e: e.memset(CST[:, 1:2], EPS), [], ["CST1"])
    P.dve(lambda e: e.memset(CST[:, 2:3], 0.0), [], ["CST2"])
    CSTK = ["CST0", "CST1", "CST2"]

    STOPS = ["mod", "gather", "w_in", "xchg", "attA", "attB", "branch", "wout", "moe_norm", "p1", "p2"]
    NEEDW = {"gather": list(WSHARD), "w_in": ["w_in", "w_gate"], "xchg": ["w_in", "w_gate"], "attA": ["w_in", "w_gate"],
             "attB": ["w_in", "w_gate"], "branch": ["w_in", "w_gate", "w_branch_a", "w_branch_b"],
             "wout": ["w_in", "w_gate", "w_branch_a", "w_branch_b", "w_out"],
             "moe_norm": ["w_in", "w_gate", "w_branch_a", "w_branch_b", "w_out"],
             "p1": ["w_in", "w_gate", "w_branch_a", "w_branch_b", "w_out", "w_exp_gate", "w_exp_up"],
             "p2": list(WSHARD), None: list(WSHARD), "mod": []}
    if SHARD_W:
        for nm in NEEDW[stop]:
            I(nm)
    P.begin_phase()
    WB = [P.sb(f"WBm{i}", [128, 32, 256], BF16) for i in range(3)]
    CALL = P.sb("CALL", [128, 128], F32)
    CACT = P.sb("CACT", [128, 128], BF16)
    MODS = P.sb("MODS", [128, 24, 4], F32)
    ABT = P.sb("ABT", [128, 24], F32)
    M4r = P.sb("M4", [128, 8, 96], F32)
    M4 = M4r[:, :, :].rearrange("p r (j b) -> p (r j) b", b=4)
    GM = P.sb("GM", [128, 32], F32)
    P.dma("sp", CALL[:, :], I("cT_all"), [], ["CALL"])
    P.act(lambda e: e.activation(out=CACT[:, :], in_=CALL[:, :], func=AF.Silu), ["CALL"], ["CACT"])
    nblk = 0
    for l in range(n_layers):
        for blk in range(12):
            wb = WB[nblk % 3]
            wk = ("WBm", nblk % 3)
            nblk += 1
            P.dma("pool", wb[:, :, :],
                  I("ada_w_s")[l, :, blk * 256:(blk + 1) * 256].rearrange("(k p) n -> p k n", p=128), [], [wk])
            for jj in range(2):
                j = blk * 2 + jj
                for k in range(32):
                    P.pe(lambda e, wb=wb, j=j, jj=jj, k=k: e.matmul(
                        PS[0][:, j * 4:(j + 1) * 4], wb[:, k, jj * 128:(jj + 1) * 128],
                        CACT[:, k * 4:(k + 1) * 4], start=(k == 0), stop=(k == 31)),
                        [wk, "CACT"], [psk(0)])
        P.dma("sp", ABT[:, :], I("ada_bT")[l], [], ["ABT"])
        psv = PS[0][:, 0:96].rearrange("p (j b) -> p j b", b=4)
        for b in range(4):
            P.dve(lambda e, b=b, psv=psv: e.tensor_tensor(out=MODS[:, :, b], in0=psv[:, :, b], in1=ABT[:, :],
                                                          op=ALU.add), [psk(0), "ABT"], ["MODS"])
        if stop == "mod_a":
            dump("d_mods", MODS[:, :, :], ["MODS"])
            P.end_phase()
            return finish()
        P.dma("sp", ccin[l], MODS[:, :, :].rearrange("p j b -> p (j b)"), ["MODS"], [("ccin", l)])
        P._add("pool", lambda e, l=l: e.collective_compute(
            "AllGather", ALU.bypass, replica_groups=ALL8, ins=[ccin[l]], outs=[ccout[l]]),
            [("ccin", l), "WALL"], [("ccout", l), "WALL"])
        P.dma("sp", M4r[:, :, :], ccout[l].rearrange("(r p) f -> p r f", p=128), [("ccout", l)], ["M4"])
        P.dve(lambda e, l=l: e.tensor_scalar(out=MOD[l][:, :], in0=M4[:, :, 0], scalar1=BSEL[:, 0:1], scalar2=None,
                                             op0=ALU.mult), ["M4", "BSEL"], [("MOD", l)])
        for b in range(1, 4):
            P.dve(lambda e, l=l, b=b: e.scalar_tensor_tensor(out=MOD[l][:, :], in0=M4[:, :, b], scalar=BSEL[:, b:b + 1],
                                                             in1=MOD[l][:, :], op0=ALU.mult, op1=ALU.add),
                  ["M4", "BSEL", ("MOD", l)], [("MOD", l)])
        P.dma("sp", GM[:, :], I("gmixT")[l], [], ["GM"])
        P.dve(lambda e, l=l: e.scalar_tensor_tensor(out=SC1[l][:, :], in0=MOD[l][:, 32:64], scalar=1.0, in1=GM[:, :],
                                                    op0=ALU.add, op1=ALU.mult), [("MOD", l), "GM"], [("SC1", l)])
        P.dma("sp", GM[:, :], I("gffnT")[l], [], ["GM"])
        P.dve(lambda e, l=l: e.scalar_tensor_tensor(out=SC2[l][:, :], in0=MOD[l][:, 128:160], scalar=1.0, in1=GM[:, :],
                                                    op0=ALU.add, op1=ALU.mult), [("MOD", l), "GM"], [("SC2", l)])
    if stop in ("mod", "gather"):
        dump("d_mod0", MOD[0][:, :], [("MOD", 0)])
        dump("d_sc1", SC1[0][:, :], [("SC1", 0)])
        P.end_phase()
        return finish()
    P.end_phase()

    def xsrc_in(k):
        return I("xT")[k * 128:(k + 1) * 128, :]

    def xsrc_xd(k):
        return XD[k]

    def norm_stage(xsrc, scale_t, shift_t, mode, l=0):
        P.begin_phase()
        XK = [P.sb(f"XK{i}", [128, T], F32) for i in range(3)]
        SQ = [P.sb(f"SQ{i}", [128, T], BF16) for i in range(2)]
        RSTD = P.sb("RSTD", [128, T], F32)
        TMP = [P.sb(f"TMP{i}", [128, T], F32) for i in range(2)]
        if mode == "moe":
            H2F = [P.sb(f"H2F{i}", [128, T], F32) for i in range(2)]
            WR = P.sb("WR", [128, 32, 36], F32)
            LT = P.sb("LT", [36, T], F32)
            LTM = P.sb("LTM", [128, 36], F32)
            BR = P.sb("BR", [128, 36], F32)
            RT = [P.sb(f"RT{i}", [128, 40], F32) for i in range(2)]
            for kq in range(4):
                P.dma("sp", WR[:, kq * 8:(kq + 1) * 8, :],
                      I("w_r")[l, kq * 1024:(kq + 1) * 1024, :].rearrange("(k p) n -> p k n", p=128),
                      ["WR"] if kq else [], ["WR"])
            P.dma("sp", BR[:, :], I("br_t")[l], [], ["BR"])
        if mode == "final":
            OT = [P.sb(f"OT{i}", [128, T], F32) for i in range(2)]
        for k in range(32):
            xk = XK[k % 3]
            xkk = ("XK", k % 3)
            sq = SQ[k % 2]
            sqk = ("SQ", k % 2)
            P.dma("sp", xk[:, :], xsrc(k), [("XD", k)], [xkk])
            P.act(lambda e, xk=xk, sq=sq: e.activation(out=sq[:, :], in_=xk[:, :], func=AF.Square), [xkk], [sqk])
            for tc in range(2):
                P.pe(lambda e, sq=sq, tc=tc, k=k: e.matmul(PS[tc][:, :], ONESb[:, :], sq[:, tc * 512:(tc + 1) * 512],
                                                           start=(k == 0), stop=(k == 31)),
                     [sqk, "ONESb"], [psk(tc)])
        for tc in range(2):
            P.act(lambda e, tc=tc: e.activation(out=RSTD[:, tc * 512:(tc + 1) * 512], in_=PS[tc][:, :], func=AF.Sqrt,
                                                bias=CST[:, 1:2], scale=1.0 / D), [psk(tc), "CST1"], [("RSTD", tc)])
            P.dve(lambda e, tc=tc: e.reciprocal(out=RSTD[:, tc * 512:(tc + 1) * 512],
                                                in_=RSTD[:, tc * 512:(tc + 1) * 512]), [("RSTD", tc)], [("RSTD", tc)])
        for k in range(32):
            xk = XK[k % 3]
            xkk = ("XK", k % 3)
            tm = TMP[k % 2]
            tmk = ("TMP", k % 2)
            P.dma("sp", xk[:, :], xsrc(k), [("XD", k)], [xkk])
            P.dve(lambda e, xk=xk, tm=tm: e.tensor_tensor(out=tm[:, :], in0=xk[:, :], in1=RSTD[:, :], op=ALU.mult),
                  [xkk, ("RSTD", 0), ("RSTD", 1)], [tmk])
            if mode == "final":
                ot = OT[k % 2]
                otk = ("OT", k % 2)
                P.act(lambda e, tm=tm, ot=ot, k=k: e.activation(out=ot[:, :], in_=tm[:, :], func=AF.Identity,
                                                                bias=CST[:, 2:3], scale=GFIN[:, k:k + 1]),
                      [tmk, "GFIN", "CST2"], [otk])
                final_ops.append(P.dma("sp", outT[k * 128:(k + 1) * 128, :], ot[:, :], [otk], [("outT", k)]))
                continue
            P.act(lambda e, tm=tm, k=k: e.activation(out=HT[:, k, :], in_=tm[:, :], func=AF.Identity,
                                                     bias=shift_t[:, k:k + 1], scale=scale_t[:, k:k + 1]),
                  [tmk, ("MOD", l), ("SC1", l), ("SC2", l)], [("HT", k)])
            if mode == "moe":
                hf = H2F[k % 2]
                hfk = ("H2F", k % 2)
                P.act(lambda e, tm=tm, hf=hf, k=k: e.activation(out=hf[:, :], in_=tm[:, :], func=AF.Identity,
                                                                bias=shift_t[:, k:k + 1], scale=scale_t[:, k:k + 1]),
                      [tmk, ("MOD", l), ("SC2", l)], [hfk])
                for tc in range(2):
                    P.pe(lambda e, hf=hf, tc=tc, k=k: e.matmul(PS[2 + tc][0:36, :], WR[:, k, :],
                                                               hf[:, tc * 512:(tc + 1) * 512],
                                                               start=(k == 0), stop=(k == 31)),
                         [hfk, "WR"], [psk(2 + tc)])
        if mode == "moe":
            for tc in range(2):
                P.act(lambda e, tc=tc: e.activation(out=LT[:, tc * 512:(tc + 1) * 512], in_=PS[2 + tc][0:36, :],
                                                    func=AF.Identity, bias=CST[0:36, 2:3], scale=1.0),
                      [psk(2 + tc), "CST2"], [("LT", tc)])
            for tt in range(8):
                pb = 4 + (tt % 2)
                rt = RT[tt % 2]
                rk = ("RT", tt % 2)
                P.pe(lambda e, tt=tt, pb=pb: e.matmul(PS[pb][:, 0:36], LT[:, tt * 128:(tt + 1) * 128], IDf[0:36, 0:36],
                                                      start=True, stop=True), [("LT", tt // 4), "IDf"], [psk(pb)])
                P.dve(lambda e, pb=pb: e.tensor_tensor(out=LTM[:, :], in0=PS[pb][:, 0:36], in1=BR[:, :], op=ALU.add),
                      [psk(pb), "BR"], ["LTM"])
                routing(tt, LTM, rt, rk)
        P.end_phase()

    def routing(tt, Lg, rt, rk):
        rd = ["LTM"]
        P.dve(lambda e: e.tensor_reduce(out=rt[:, 0:1], in_=Lg[:, 0:4], axis=AX.X, op=ALU.max), rd, [rk])
        P.dve(lambda e: e.tensor_scalar(out=rt[:, 1:2], in0=rt[:, 0:1], scalar1=-1.0, scalar2=None, op0=ALU.mult),
              [rk], [rk])
        P.dve(lambda e: e.tensor_scalar(out=rt[:, 4:8], in0=Lg[:, 0:4], scalar1=rt[:, 0:1], scalar2=None,
                                        op0=ALU.is_equal), rd + [rk], [rk])
        P.act(lambda e: e.activation(out=rt[:, 16:20], in_=Lg[:, 0:4], func=AF.Exp, bias=rt[:, 1:2], scale=1.0),
              rd + [rk], [rk])
        P.dve(lambda e: e.tensor_reduce(out=rt[:, 2:3], in_=rt[:, 16:20], axis=AX.X, op=ALU.add), [rk], [rk])
        P.dve(lambda e: e.reciprocal(out=rt[:, 3:4], in_=rt[:, 2:3]), [rk], [rk])
        P.dve(lambda e: e.tensor_scalar(out=rt[:, 8:16], in0=Lg[:, 4:12], scalar1=rt[:, 4:5], scalar2=None,
                                        op0=ALU.mult), rd + [rk], [rk])
        for g in range(1, 4):
            P.dve(lambda e, g=g: e.scalar_tensor_tensor(out=rt[:, 8:16], in0=Lg[:, 4 + 8 * g:12 + 8 * g],
                                                        scalar=rt[:, 4 + g:5 + g], in1=rt[:, 8:16],
                                                        op0=ALU.mult, op1=ALU.add), rd + [rk], [rk])
        P.dve(lambda e: e.tensor_reduce(out=rt[:, 32:33], in_=rt[:, 8:16], axis=AX.X, op=ALU.max), [rk], [rk])
        P.dve(lambda e: e.tensor_scalar(out=rt[:, 16:24], in0=rt[:, 8:16], scalar1=rt[:, 32:33], scalar2=None,
                                        op0=ALU.is_equal), [rk], [rk])
        P.dve(lambda e: e.scalar_tensor_tensor(out=rt[:, 24:32], in0=rt[:, 16:24], scalar=-1e30, in1=rt[:, 8:16],
                                               op0=ALU.mult, op1=ALU.add), [rk], [rk])
        P.dve(lambda e: e.tensor_reduce(out=rt[:, 33:34], in_=rt[:, 24:32], axis=AX.X, op=ALU.max), [rk], [rk])
        P.dve(lambda e: e.tensor_scalar(out=rt[:, 24:32], in0=rt[:, 24:32], scalar1=rt[:, 33:34], scalar2=None,
                                        op0=ALU.is_equal), [rk], [rk])
        P.dve(lambda e: e.tensor_tensor(out=rt[:, 34:35], in0=rt[:, 33:34], in1=rt[:, 32:33], op=ALU.subtract),
              [rk], [rk])
        P.act(lambda e: e.activation(out=rt[:, 34:35], in_=rt[:, 34:35], func=AF.Exp, bias=CST[:, 2:3], scale=1.0),
              [rk, "CST2"], [rk])
        P.dve(lambda e: e.tensor_scalar(out=rt[:, 35:36], in0=rt[:, 34:35], scalar1=1.0, scalar2=None, op0=ALU.add),
              [rk], [rk])
        P.dve(lambda e: e.reciprocal(out=rt[:, 35:36], in_=rt[:, 35:36]), [rk], [rk])
        P.dve(lambda e: e.tensor_tensor(out=rt[:, 36:37], in0=rt[:, 34:35], in1=rt[:, 35:36], op=ALU.mult),
              [rk], [rk])
        P.dve(lambda e: e.tensor_tensor(out=rt[:, 37:38], in0=rt[:, 35:36], in1=rt[:, 3:4], op=ALU.mult), [rk], [rk])
        P.dve(lambda e: e.tensor_tensor(out=rt[:, 38:39], in0=rt[:, 36:37], in1=rt[:, 3:4], op=ALU.mult), [rk], [rk])
        P.dve(lambda e: e.tensor_scalar(out=rt[:, 16:24], in0=rt[:, 16:24], scalar1=rt[:, 37:38], scalar2=None,
                                        op0=ALU.mult), [rk], [rk])
        P.dve(lambda e: e.scalar_tensor_tensor(out=rt[:, 16:24], in0=rt[:, 24:32], scalar=rt[:, 38:39],
                                               in1=rt[:, 16:24], op0=ALU.mult, op1=ALU.add), [rk], [rk])
        for g in range(4):
            P.dve(lambda e, g=g: e.tensor_scalar(out=COMB[:, tt, 8 * g:8 * g + 8], in0=rt[:, 16:24],
                                                 scalar1=rt[:, 4 + g:5 + g], scalar2=None, op0=ALU.mult),
                  [rk], [("COMB", tt)])

    def stream_cols(wsrc_fn, nblocks, consume, WB, wctr):
        for i in range(nblocks):
            wb = WB[wctr[0] % 3]
            wk = ("WB", wctr[0] % 3)
            wctr[0] += 1
            for dst_fn, src in wsrc_fn(i):
                P.dma("pool", dst_fn(wb), src, [], [wk])
            consume(i, wb, wk)

    evc = [0]

    def evac(ps_ap, pskey, out_ap, outkey, extra_reads=()):
        evc[0] += 1
        if evc[0] % 2:
            P.act(lambda e: e.activation(out=out_ap, in_=ps_ap, func=AF.Identity, bias=CST[:, 2:3], scale=1.0),
                  [pskey, "CST2"] + list(extra_reads), [outkey])
        else:
            P.dve(lambda e: e.tensor_copy(out=out_ap, in_=ps_ap), [pskey] + list(extra_reads), [outkey])

    def layer(l):
        xsrc = xsrc_in if l == 0 else xsrc_xd
        norm_stage(xsrc, SC1[l], MOD[l][:, 0:32], "h", l)
        P.begin_phase()
        WB = [P.sb(f"WB{i}", [128, 32, 256], BF16) for i in range(3)]
        EV = [P.sb(f"EV{i}", [128, 512], BF16) for i in range(4)]
        WF = P.sb("WF", [128, 32, 16], BF16)
        BFT = P.sb("BFT", [128, 16], F32)
        LX = [P.sb(f"LX{i}", [128, 16], F32) for i in range(2)]
        LA = [P.sb(f"LA{i}", [128, 16], F32) for i in range(2)]
        LM = [P.sb(f"LM{i}", [128, 16], F32) for i in range(2)]
        BGT = P.sb("BGT", [128, 64], F32)
        P.dma("sp", BFT[:, :], I("bf_t")[l], [], ["BFT"])
        P.dma("sp", BGT[:, :], I("bgT")[l], [], ["BGT"])
        wctr = [0]
        psc = [0]
        evi = [0]
        uq = [0]

        def ukey():
            uq[0] += 1
            return ("u", uq[0])

        VA_view = XBi[l][4096:6144, :].rearrange("(t a) c -> t (a c)", a=2)
        VB_view = XBi[l][6144:8192, :].rearrange("(t a) c -> t (a c)", a=2)
        w_in_l = I("w_in")
        segs = [("qa", 0), ("ka", 2048), ("va", 4096), ("qb", 6160), ("kb", 8208), ("vb", 10256)]
        for seg, c0 in segs:
            for bi in range(8):
                wb = WB[wctr[0] % 3]
                wk = ("WB", wctr[0] % 3)
                wctr[0] += 1
                P.dma("pool", wb[:, :, :],
                      w_in_l[l, :, c0 + bi * 256:c0 + (bi + 1) * 256].rearrange("(k p) n -> p k n", p=128), [], [wk])
                if seg in ("qa", "ka", "qb", "kb"):
                    for hh in range(2):
                        h = bi * 2 + hh
                        for tc in range(2):
                            pb = psc[0] % 4
                            psc[0] += 1
                            for k in range(32):
                                P.pe(lambda e, wb=wb, hh=hh, tc=tc, k=k, pb=pb: e.matmul(
                                    PS[pb][:, :], wb[:, k, hh * 128:(hh + 1) * 128], HT[:, k, tc * 512:(tc + 1) * 512],
                                    start=(k == 0), stop=(k == 31)), [wk, ("HT", k)], [psk(pb)])
                            ev = EV[evi[0] % 4]
                            evk = ("EV", evi[0] % 4)
                            evi[0] += 1
                            evac(PS[pb][:, :], psk(pb), ev[:, :], evk)
                            if seg == "qa":
                                dst = QTa[h][:, tc * 512:(tc + 1) * 512]
                            elif seg == "qb":
                                dst = QTb[h][:, tc * 512:(tc + 1) * 512]
                            elif seg == "ka":
                                dst = XBi[l][h * 128:(h + 1) * 128, tc * 512:(tc + 1) * 512]
                            else:
                                dst = XBi[l][2048 + h * 128:2048 + (h + 1) * 128, tc * 512:(tc + 1) * 512]
                            P.dma("sp", dst, ev[:, :], [evk], [ukey()])
                else:
                    vview = VA_view if seg == "va" else VB_view
                    for tt in range(8):
                        pb = psc[0] % 4
                        psc[0] += 1
                        for k in range(32):
                            P.pe(lambda e, wb=wb, tt=tt, k=k, pb=pb: e.matmul(
                                PS[pb][:, 0:256], HT[:, k, tt * 128:(tt + 1) * 128], wb[:, k, :],
                                start=(k == 0), stop=(k == 31)), [wk, ("HT", k)], [psk(pb)])
                        ev = EV[evi[0] % 4]
                        evk = ("EV", evi[0] % 4)
                        evi[0] += 1
                        evac(PS[pb][:, 0:256], psk(pb), ev[:, 0:256], evk)
                        P.dma("sp", vview[tt * 128:(tt + 1) * 128, bi * 256:(bi + 1) * 256], ev[:, 0:256],
                              [evk], [ukey()])
        for kq in range(4):
            P.dma("pool", WF[:, kq * 8:(kq + 1) * 8, :],
                  w_in_l[l, kq * 1024:(kq + 1) * 1024, 6144:6160].rearrange("(k p) n -> p k n", p=128),
                  ["WF"] if kq else [], ["WF"])
        for tt in range(8):
            pb = 4 + tt % 2
            i2 = tt % 2
            for k in range(32):
                P.pe(lambda e, tt=tt, k=k, pb=pb: e.matmul(PS[pb][:, 0:16], HT[:, k, tt * 128:(tt + 1) * 128], WF[:, k, :],
                                                          start=(k == 0), stop=(k == 31)), ["WF", ("HT", k)], [psk(pb)])
            lx, la, lm = LX[i2], LA[i2], LM[i2]
            P.dve(lambda e, pb=pb, lx=lx: e.tensor_tensor(out=lx[:, :], in0=PS[pb][:, 0:16], in1=BFT[:, :], op=ALU.add),
                  [psk(pb), "BFT"], [("LX", i2)])
            P.act(lambda e, lx=lx, la=la: e.activation(out=la[:, :], in_=lx[:, :], func=AF.Abs, bias=CST[:, 2:3], scale=1.0),
                  [("LX", i2), "CST2"], [("LA", i2)])
            P.act(lambda e, la=la: e.activation(out=la[:, :], in_=la[:, :], func=AF.Exp, bias=CST[:, 2:3], scale=-1.0),
                  [("LA", i2), "CST2"], [("LA", i2)])
            P.act(lambda e, la=la: e.activation(out=la[:, :], in_=la[:, :], func=AF.Ln, bias=CST[:, 0:1], scale=1.0),
                  [("LA", i2), "CST0"], [("LA", i2)])
            P.dve(lambda e, lx=lx, lm=lm: e.tensor_scalar(out=lm[:, :], in0=lx[:, :], scalar1=0.0, scalar2=None,
                                                          op0=ALU.min), [("LX", i2)], [("LM", i2)])
            P.dve(lambda e, lm=lm, la=la, tt=tt: e.tensor_tensor(out=LFO[:, tt, :], in0=lm[:, :], in1=la[:, :],
                                                                 op=ALU.subtract),
                  [("LM", i2), ("LA", i2)], [("LFO", tt)])
        P.dma("sp", LFi[l], LFO[:, :, :].rearrange("p a b -> p (a b)"), [("LFO", tt) for tt in range(8)], [ukey()])
        w_gate_l = I("w_gate")
        for gb in range(32):
            wb = WB[wctr[0] % 3]
            wk = ("WB", wctr[0] % 3)
            wctr[0] += 1
            P.dma("pool", wb[:, :, :], w_gate_l[l, :, gb * 256:(gb + 1) * 256].rearrange("(k p) n -> p k n", p=128),
                  [], [wk])
            for cc in range(2):
                j = gb * 2 + cc
                for tc in range(2):
                    pb = psc[0] % 4
                    psc[0] += 1
                    for k in range(32):
                        P.pe(lambda e, wb=wb, cc=cc, tc=tc, k=k, pb=pb: e.matmul(
                            PS[pb][:, :], wb[:, k, cc * 128:(cc + 1) * 128], HT[:, k, tc * 512:(tc + 1) * 512],
                            start=(k == 0), stop=(k == 31)), [wk, ("HT", k)], [psk(pb)])
                    ev = EV[evi[0] % 4]
                    evk = ("EV", evi[0] % 4)
                    evi[0] += 1
                    P.act(lambda e, pb=pb, ev=ev, j=j: e.activation(out=ev[:, :], in_=PS[pb][:, :], func=AF.Sigmoid,
                                                                   bias=BGT[:, j:j + 1], scale=1.0),
                          [psk(pb), "BGT"], [evk])
                    P.dma("sp", GD[j][:, tc * 512:(tc + 1) * 512], ev[:, :], [evk], [ukey()])
        P.end_phase()

        if stop == "w_in":
            P.begin_phase()
            DBG = P.sb("DBG", [128, 1024], F32)
            dump("d_out", LFO[:, :, :].rearrange("p a b -> p (a b)"), [])
            P.end_phase()
            return "stop"
        for ch in range(8):
            P._add("pool", lambda e, l=l, ch=ch: e.collective_compute(
                "AllGather", ALU.bypass, replica_groups=PAIRS, ins=[XBi[l][ch * 1024:(ch + 1) * 1024, :]],
                outs=[XBo[l][ch]]), [("XBo", l)], [("XBo", l)])
        P._add("pool", lambda e, l=l: e.collective_compute(
            "AllGather", ALU.bypass, replica_groups=PAIRS, ins=[LFi[l]], outs=[LFo[l]]), [("XBo", l)], [("LFo", l)])
        P.begin_phase()
        LFP = P.sb("LFP", [128, 8, 16], F32)
        P.dma("sp", LFP[:, :, :], LFo[l][0:128, :].rearrange("p (a b) -> p a b", b=16), [("LFo", l)], ["LFP"])

        def lfsrc(i):
            return (LFP[:, i, :], "LFP") if i < 8 else (LFO[:, i - 8, :], ("LFO", i - 8))
        for i in range(16):
            pb = i % 4
            for i2 in range(i + 1):
                src, sk = lfsrc(i2)
                lhs = TRI if i2 == i else ONESf
                P.pe(lambda e, pb=pb, lhs=lhs, src=src, i2=i2, i=i: e.matmul(PS[pb][:, 0:16], lhs[:, :], src,
                                                                            start=(i2 == 0), stop=(i2 == i)),
                     [sk, "TRI", "ONESf"], [psk(pb)])
            P.dve(lambda e, pb=pb, i=i: e.tensor_copy(out=CUM[:, i, :], in_=PS[pb][:, 0:16]), [psk(pb)], [("CUM", i)])
            if i < 8:
                P.dve(lambda e, pb=pb, i=i: e.tensor_scalar(out=NEGC[:, i, :], in0=PS[pb][:, 0:16], scalar1=-1.0,
                                                            scalar2=PEN[:, 0:1], op0=ALU.mult, op1=ALU.add),
                      [psk(pb), "PEN"], [("NEGC", i)])
            else:
                P.dve(lambda e, pb=pb, i=i: e.tensor_scalar(out=NEGC[:, i, :], in0=PS[pb][:, 0:16], scalar1=-1.0,
                                                            scalar2=None, op0=ALU.mult), [psk(pb)], [("NEGC", i)])
        P.end_phase()

        if stop == "xchg":
            P.begin_phase()
            DBG = P.sb("DBG", [128, 1024], F32)
            dump("d_out", CUM[:, :, :].rearrange("p a b -> p (a b)"), [])
            P.end_phase()
            return "stop"
        def finish_o(ob, ON, RINV, ctr, YT, h, qblk):
            i2 = ctr[0] % 2
            sl = (ctr[0] % 8) * 128
            ctr[0] += 1
            on, rv = ON[i2], RINV[i2]
            P.dve(lambda e: e.reciprocal(out=rv[:, :], in_=PS[ob][:, 128:129]), [psk(ob)], [("RINV", i2)])
            P.dve(lambda e: e.tensor_scalar(out=on[:, :], in0=PS[ob][:, 0:128], scalar1=rv[:, 0:1], scalar2=None,
                                            op0=ALU.mult), [psk(ob), ("RINV", i2)], [("ON", i2)])
            P.pe(lambda e: e.transpose(PSB[:, sl:sl + 128], on[:, :], IDb[:, :]), [("ON", i2), "IDb"], ["psb"])
            P.act(lambda e: e.activation(out=YT[:, h, qblk * 128:(qblk + 1) * 128], in_=PSB[:, sl:sl + 128],
                                         func=AF.Identity, bias=CST[:, 2:3], scale=1.0),
                  ["psb", "CST2"], [("YT", id(YT), h, qblk)])

        mid = contextlib.ExitStack()
        YTA = mid.enter_context(nc.sbuf_tensor(f"YTA{l}", [128, 16, T], BF16))
        YTB = mid.enter_context(nc.sbuf_tensor(f"YTB{l}", [128, 16, T], BF16))
        P.begin_phase()
        QT = [P.sb(f"QT{i}", [128, T], BF16) for i in range(2)]
        KT = [P.sb(f"KT{i}", [128, 2048], BF16) for i in range(2)]
        VA = [P.sb(f"VA{i}", [128, 16, 129], BF16) for i in range(2)]
        CQB = [P.sb(f"CQB{i}", [128, T], F32) for i in range(2)]
        DG = [P.sb(f"DG{i}", [128, 128], F32) for i in range(2)]
        TMPA = [P.sb(f"TMPA{i}", [128, 512], F32) for i in range(3)]
        PT = [P.sb(f"PT{i}", [128, 512], BF16) for i in range(3)]
        ON = [P.sb(f"ON{i}", [128, 128], BF16) for i in range(2)]
        RINV = [P.sb(f"RINV{i}", [128, 1], F32) for i in range(2)]
        for i2 in range(2):
            P.dve(lambda e, i2=i2: e.memset(VA[i2][:, :, 128:129], 1.0), [], [("VAone", i2)])
        pVa_c = [XBo[l][4 + j][0:1024, :].rearrange("(t a) c -> t (a c)", a=2) for j in range(2)]
        oVa = XBi[l][4096:6144, :].rearrange("(t a) c -> t (a c)", a=2)
        sctr = [0]
        tctr = [0]
        octr = [0]
        for h in range(16):
            b2 = h % 2
            P.dma("sp", QT[b2][:, :], QTa[h], [], [("QT", b2)])
            P.dma("sp", KT[b2][:, 0:1024], XBo[l][h // 8][(h % 8) * 128:(h % 8 + 1) * 128, :], [("XBo", l)], [("KT", b2)])
            P.dma("sp", KT[b2][:, 1024:2048], XBi[l][h * 128:(h + 1) * 128, :], [("KT", b2)], [("KT", b2)])
            P.dma("sp", VA[b2][:, 0:4, 0:128], pVa_c[0][:, h * 128:(h + 1) * 128].rearrange("(i p) c -> p i c", p=128),
                  [("XBo", l), ("VAone", b2)], [("VA", b2)])
            P.dma("sp", VA[b2][:, 4:8, 0:128], pVa_c[1][:, h * 128:(h + 1) * 128].rearrange("(i p) c -> p i c", p=128),
                  [("VA", b2)], [("VA", b2)])
            P.dma("sp", VA[b2][:, 8:16, 0:128], oVa[:, h * 128:(h + 1) * 128].rearrange("(i p) c -> p i c", p=128),
                  [("VA", b2)], [("VA", b2)])
            for half in range(2):
                for t4 in range(4):
                    tt = half * 4 + t4
                    dg = DG[tt % 2]
                    P.dve(lambda e, dg=dg, tt=tt, h=h: e.tensor_scalar(out=dg[:, :], in0=IDf[:, :],
                                                                      scalar1=CUM[:, 8 + tt, h:h + 1], scalar2=None,
                                                                      op0=ALU.mult), ["IDf"], [("DG", tt % 2)])
                    P.pe(lambda e, dg=dg, t4=t4: e.matmul(PS[6][:, t4 * 128:(t4 + 1) * 128], ONESf[:, :], dg[:, :],
                                                          start=True, stop=True), [("DG", tt % 2), "ONESf"], [psk(6)])
                P.act(lambda e, b2=b2, half=half: e.activation(out=CQB[b2][:, half * 512:(half + 1) * 512],
                                                               in_=PS[6][:, :], func=AF.Identity, bias=CST[:, 2:3],
                                                               scale=1.0), [psk(6), "CST2"], [("CQB", b2, half)])
            for j in range(2):
                blocks = [(i, 512 * j) for i in range(8)] + [(8 + kb, max(512 * j, 128 * kb)) for kb in range(4 * j + 4)]
                for (i, qs) in blocks:
                    N = 512 * j + 512 - qs
                    sbk = sctr[0] % 2
                    sctr[0] += 1
                    ti = tctr[0] % 3
                    tctr[0] += 1
                    tm, pt = TMPA[ti], PT[ti]
                    P.pe(lambda e, sbk=sbk, N=N, b2=b2, i=i, qs=qs: e.matmul(
                        PS[sbk][:, 0:N], KT[b2][:, i * 128:(i + 1) * 128], QT[b2][:, qs:qs + N], start=True, stop=True),
                        [("KT", b2), ("QT", b2)], [psk(sbk)])
                    P.dve(lambda e, sbk=sbk, N=N, tm=tm, b2=b2, qs=qs: e.scalar_tensor_tensor(
                        out=tm[:, 0:N], in0=PS[sbk][:, 0:N], scalar=SCALE, in1=CQB[b2][:, qs:qs + N],
                        op0=ALU.mult, op1=ALU.add), [psk(sbk), ("CQB", b2, 0), ("CQB", b2, 1)], [("TMPA", ti)])
                    if i >= 8 and 128 * (i - 8) == qs:
                        P.dve(lambda e, tm=tm: e.tensor_tensor(out=tm[:, 0:128], in0=tm[:, 0:128], in1=CMASK[:, :],
                                                               op=ALU.add), [("TMPA", ti), "CMASK"], [("TMPA", ti)])
                    P.act(lambda e, tm=tm, pt=pt, N=N, i=i, h=h: e.activation(
                        out=pt[:, 0:N], in_=tm[:, 0:N], func=AF.Exp, bias=NEGC[:, i, h:h + 1], scale=1.0),
                        [("TMPA", ti)], [("PT", ti)])
                    for s in range(N // 128):
                        qblk = (qs + s * 128) // 128
                        ob = 2 + (qblk % 4)
                        P.pe(lambda e, pt=pt, s=s, ob=ob, b2=b2, i=i, qblk=qblk: e.matmul(
                            PS[ob][:, 0:129], pt[:, s * 128:(s + 1) * 128], VA[b2][:, i, :],
                            start=(i == 0), stop=(i == 8 + qblk)), [("PT", ti), ("VA", b2)], [psk(ob)])
                for s4 in range(4):
                    finish_o(2 + s4, ON, RINV, octr, YTA, h, 4 * j + s4)
        P.end_phase()

        if stop == "attA":
            P.begin_phase()
            DBG = P.sb("DBG", [128, 1024], F32)
            dump("d_out", YTA[:, 0, :], [])
            P.end_phase()
            mid.close()
            return "stop"
        P.begin_phase()
        QB = [P.sb(f"QB{i}", [128, T], BF16) for i in range(2)]
        KB = [P.sb(f"KB{i}", [128, 1536], BF16) for i in range(2)]
        VB = [P.sb(f"VB{i}", [128, 12, 129], BF16) for i in range(2)]
        BMr = [P.sb(f"BMr{i}", [128, 640], F32) for i in range(2)]
        BMm = [P.sb(f"BMm{i}", [128, 640], F32) for i in range(2)]
        BMP = [P.sb(f"BMP{i}", [128, 640], F32) for i in range(2)]
        TMPB = [P.sb(f"TMPB{i}", [128, 640], F32) for i in range(2)]
        PTB = [P.sb(f"PTB{i}", [128, 640], BF16) for i in range(2)]
        ON = [P.sb(f"ONb{i}", [128, 128], BF16) for i in range(2)]
        RINV = [P.sb(f"RINVb{i}", [128, 1], F32) for i in range(2)]
        for i2 in range(2):
            P.dve(lambda e, i2=i2: e.memset(VB[i2][:, :, 128:129], 1.0), [], [("VBone", i2)])
        pVb = XBo[l][7][0:1024, :].rearrange("(t a) c -> t (a c)", a=2)
        oVb = XBi[l][6144:8192, :].rearrange("(t a) c -> t (a c)", a=2)
        relT = I("relT")
        mctr = [0]
        for h in range(16):
            b2 = h % 2
            P.dma("sp", QB[b2][:, :], QTb[h], [], [("QB", b2)])
            P.dma("sp", KB[b2][:, 0:512], XBo[l][2 + h // 8][(h % 8) * 128:(h % 8 + 1) * 128, 512:1024], [], [("KB", b2)])
            P.dma("sp", KB[b2][:, 512:1536], XBi[l][2048 + h * 128:2048 + (h + 1) * 128, :], [("KB", b2)], [("KB", b2)])
            P.dma("sp", VB[b2][:, 0:4, 0:128],
                  pVb[:, h * 128:(h + 1) * 128].rearrange("(i p) c -> p i c", p=128), [("VBone", b2)], [("VB", b2)])
            P.dma("sp", VB[b2][:, 4:12, 0:128], oVb[:, h * 128:(h + 1) * 128].rearrange("(i p) c -> p i c", p=128),
                  [("VB", b2)], [("VB", b2)])
            P.dma("sp", BMr[b2][:, :], relT[l, h], [], [("BMr", b2)])
            P.dve(lambda e, b2=b2: e.tensor_tensor(out=BMm[b2][:, :], in0=BMr[b2][:, :], in1=BMASK[:, :], op=ALU.add),
                  [("BMr", b2), "BMASK"], [("BMm", b2)])
            P.dve(lambda e, b2=b2: e.tensor_scalar(out=BMP[b2][:, :], in0=BMm[b2][:, :], scalar1=PEN[:, 0:1],
                                                   scalar2=None, op0=ALU.add), [("BMm", b2), "PEN"], [("BMP", b2)])
            for m in range(8):
                mi = mctr[0] % 2
                mctr[0] += 1
                pa, pbk, ob = mi, 2 + mi, 4 + mi
                tm, pt = TMPB[mi], PTB[mi]
                for a in range(4):
                    P.pe(lambda e, pa=pa, a=a, m=m, b2=b2: e.matmul(
                        PS[pa][:, a * 128:(a + 1) * 128], KB[b2][:, (m + a) * 128:(m + a + 1) * 128],
                        QB[b2][:, m * 128:(m + 1) * 128], start=True, stop=True), [("KB", b2), ("QB", b2)], [psk(pa)])
                P.pe(lambda e, pbk=pbk, m=m, b2=b2: e.matmul(
                    PS[pbk][:, 0:128], KB[b2][:, (m + 4) * 128:(m + 5) * 128], QB[b2][:, m * 128:(m + 1) * 128],
                    start=True, stop=True), [("KB", b2), ("QB", b2)], [psk(pbk)])
                npv = max(0, 4 - m) * 128
                if npv > 0:
                    P.dve(lambda e, pa=pa, tm=tm, npv=npv, b2=b2: e.scalar_tensor_tensor(
                        out=tm[:, 0:npv], in0=PS[pa][:, 0:npv], scalar=SCALE, in1=BMP[b2][:, 0:npv],
                        op0=ALU.mult, op1=ALU.add), [psk(pa), ("BMP", b2)], [("TMPB", mi)])
                if npv < 512:
                    P.dve(lambda e, pa=pa, tm=tm, npv=npv, b2=b2: e.scalar_tensor_tensor(
                        out=tm[:, npv:512], in0=PS[pa][:, npv:512], scalar=SCALE, in1=BMm[b2][:, npv:512],
                        op0=ALU.mult, op1=ALU.add), [psk(pa), ("BMm", b2)], [("TMPB", mi)])
                P.dve(lambda e, pbk=pbk, tm=tm, b2=b2: e.scalar_tensor_tensor(
                    out=tm[:, 512:640], in0=PS[pbk][:, 0:128], scalar=SCALE, in1=BMm[b2][:, 512:640],
                    op0=ALU.mult, op1=ALU.add), [psk(pbk), ("BMm", b2)], [("TMPB", mi)])
                P.act(lambda e, tm=tm, pt=pt: e.activation(out=pt[:, :], in_=tm[:, :], func=AF.Exp, bias=CST[:, 2:3],
                                                           scale=1.0), [("TMPB", mi), "CST2"], [("PTB", mi)])
                for a in range(5):
                    P.pe(lambda e, ob=ob, pt=pt, a=a, m=m, b2=b2: e.matmul(
                        PS[ob][:, 0:129], pt[:, a * 128:(a + 1) * 128], VB[b2][:, m + a, :],
                        start=(a == 0), stop=(a == 4)), [("PTB", mi), ("VB", b2)], [psk(ob)])
                finish_o(ob, ON, RINV, octr, YTB, h, m)
        P.end_phase()

        if stop == "attB":
            P.begin_phase()
            DBG = P.sb("DBG", [128, 1024], F32)
            dump("d_out", YTB[:, 0, :], [])
            P.end_phase()
            mid.close()
            return "stop"
        P.begin_phase()
        WB = [P.sb(f"WBb{i}", [128, 32, 256], BF16) for i in range(2)]
        GA = [P.sb(f"GA{i}", [128, 2, T], BF16) for i in range(2)]
        GBt = [P.sb(f"GB{i}", [128, 2, T], BF16) for i in range(2)]
        T1 = [P.sb(f"T1{i}", [128, 512], F32) for i in range(2)]
        T2 = [P.sb(f"T2{i}", [128, 512], F32) for i in range(2)]
        wa_l, wb_l = I("w_branch_a"), I("w_branch_b")
        cnt = [0]
        for c2 in range(16):
            wb = WB[c2 % 2]
            wk = ("WB", c2 % 2)
            g2 = c2 % 2
            P.dma("pool", wb[:, 0:16, :], wa_l[l, :, c2 * 256:(c2 + 1) * 256].rearrange("(k p) n -> p k n", p=128),
                  [], [wk])
            P.dma("pool", wb[:, 16:32, :], wb_l[l, :, c2 * 256:(c2 + 1) * 256].rearrange("(k p) n -> p k n", p=128),
                  [wk], [wk])
            P.dma("sp", GA[g2][:, :, :], GD[c2 * 2:c2 * 2 + 2].rearrange("j p t -> p j t"), [], [("GA", g2)])
            P.dma("sp", GBt[g2][:, :, :], GD[32 + c2 * 2:32 + c2 * 2 + 2].rearrange("j p t -> p j t"), [], [("GB", g2)])
            for cc in range(2):
                for tc in range(2):
                    ci = cnt[0] % 2
                    cnt[0] += 1
                    pA, pB = ci, 2 + ci
                    for k in range(16):
                        P.pe(lambda e, wb=wb, cc=cc, tc=tc, k=k, pA=pA: e.matmul(
                            PS[pA][:, :], wb[:, k, cc * 128:(cc + 1) * 128], YTA[:, k, tc * 512:(tc + 1) * 512],
                            start=(k == 0), stop=(k == 15)), [wk], [psk(pA)])
                    for k in range(16):
                        P.pe(lambda e, wb=wb, cc=cc, tc=tc, k=k, pB=pB: e.matmul(
                            PS[pB][:, :], wb[:, 16 + k, cc * 128:(cc + 1) * 128], YTB[:, k, tc * 512:(tc + 1) * 512],
                            start=(k == 0), stop=(k == 15)), [wk], [psk(pB)])
                    t1, t2 = T1[ci], T2[ci]
                    P.dve(lambda e, t1=t1, pA=pA, g2=g2, cc=cc, tc=tc: e.tensor_tensor(
                        out=t1[:, :], in0=PS[pA][:, :], in1=GA[g2][:, cc, tc * 512:(tc + 1) * 512], op=ALU.mult),
                        [psk(pA), ("GA", g2)], [("T1", ci)])
                    P.dve(lambda e, t2=t2, pB=pB, g2=g2, cc=cc, tc=tc: e.tensor_tensor(
                        out=t2[:, :], in0=PS[pB][:, :], in1=GBt[g2][:, cc, tc * 512:(tc + 1) * 512], op=ALU.mult),
                        [psk(pB), ("GB", g2)], [("T2", ci)])
                    kc = c2 * 2 + cc
                    P.dve(lambda e, t1=t1, t2=t2, kc=kc, tc=tc: e.tensor_tensor(
                        out=HT[:, kc, tc * 512:(tc + 1) * 512], in0=t1[:, :], in1=t2[:, :], op=ALU.add),
                        [("T1", ci), ("T2", ci)], [("HT", kc)])
        P.end_phase()

        if stop == "branch":
            P.begin_phase()
            dump("d_out", HT[:, 0, :], [])
            P.end_phase()
            mid.close()
            return "stop"
        mid.close()

        P.begin_phase()
        WB = [P.sb(f"WBo{i}", [128, 32, 256], BF16) for i in range(3)]
        XK = [P.sb(f"XKo{i}", [128, T], F32) for i in range(2)]
        XN = [P.sb(f"XNo{i}", [128, T], F32) for i in range(2)]
        w_out_l = I("w_out")
        cnt = [0]
        for c2 in range(16):
            wb = WB[c2 % 3]
            wk = ("WB", c2 % 3)
            P.dma("pool", wb[:, :, :], w_out_l[l, :, c2 * 256:(c2 + 1) * 256].rearrange("(k p) n -> p k n", p=128),
                  [], [wk])
            for cc in range(2):
                kc = c2 * 2 + cc
                x2 = kc % 2
                P.dma("sp", XK[x2][:, :], xsrc(kc), [], [("XK", x2)])
                for tc in range(2):
                    pb = cnt[0] % 4
                    cnt[0] += 1
                    for k in range(32):
                        P.pe(lambda e, wb=wb, cc=cc, tc=tc, k=k, pb=pb: e.matmul(
                            PS[pb][:, :], wb[:, k, cc * 128:(cc + 1) * 128], HT[:, k, tc * 512:(tc + 1) * 512],
                            start=(k == 0), stop=(k == 31)), [wk], [psk(pb)])
                    P.dve(lambda e, pb=pb, x2=x2, kc=kc, tc=tc, l=l: e.scalar_tensor_tensor(
                        out=XN[x2][:, tc * 512:(tc + 1) * 512], in0=PS[pb][:, :], scalar=MOD[l][:, 64 + kc:65 + kc],
                        in1=XK[x2][:, tc * 512:(tc + 1) * 512], op0=ALU.mult, op1=ALU.add),
                        [psk(pb), ("XK", x2)], [("XN", x2, tc)])
                P.dma("sp", XD[kc], XN[x2][:, :], [("XN", x2, 0), ("XN", x2, 1)], [("XN", x2, 0), ("XN", x2, 1)])
        P.end_phase()

        if stop == "wout":
            P.begin_phase()
            DBG = P.sb("DBG", [128, 1024], F32)
            P.dma("sp", DBG[:, :], XD[0], [], ["DBG"])
            dump("d_out", DBG[:, :], ["DBG"])
            P.end_phase()
            return "stop"
        norm_stage(xsrc_xd, SC2[l], MOD[l][:, 96:128], "moe", l)

        if stop == "moe_norm":
            P.begin_phase()
            dump("d_out", COMB[:, :, :].rearrange("p a b -> p (a b)"), [])
            P.end_phase()
            return "stop"
        P.begin_phase()
        WB = [P.sb(f"WBe{i}", [128, 32, 256], BF16) for i in range(4)]
        CT = P.sb("CT", [32, T], F32)
        RS = [P.sb(f"RS{i}", [32, T], F32) for i in range(2)]
        CB = [P.sb(f"CB{i}", [128, T], F32) for i in range(2)]
        SG = [P.sb(f"SG{i}", [128, 512], F32) for i in range(2)]
        TU = [P.sb(f"TU{i}", [128, 512], F32) for i in range(2)]
        AT = [P.sb(f"AT{i}", [128, 512], BF16) for i in range(3)]
        for tt in range(8):
            pb = 4 + tt % 2
            P.pe(lambda e, tt=tt, pb=pb: e.matmul(PS[pb][0:32, 0:128], COMB[:, tt, :], IDf[:, :], start=True, stop=True),
                 [("COMB", tt), "IDf"], [psk(pb)])
            P.dve(lambda e, tt=tt, pb=pb: e.tensor_copy(out=CT[:, tt * 128:(tt + 1) * 128], in_=PS[pb][0:32, 0:128]),
                  [psk(pb)], ["CT"])
        weg, weu = I("w_exp_gate"), I("w_exp_up")
        wc = [0]
        cnt = [0]
        ai = [0]
        for ex in range(32):
            g, ee = ex // 8, ex % 8
            c2i = ex % 2
            rs, cb = RS[c2i], CB[c2i]
            P.dve(lambda e, rs=rs, ex=ex: e.tensor_scalar(out=rs[:, :], in0=CT[:, :], scalar1=IDf[0:32, ex:ex + 1],
                                                          scalar2=None, op0=ALU.mult), ["CT", "IDf"], [("RS", c2i)])
            for tc in range(2):
                P.pe(lambda e, rs=rs, tc=tc: e.matmul(PS[6][:, :], ONESf[0:32, :], rs[:, tc * 512:(tc + 1) * 512],
                                                      start=True, stop=True), [("RS", c2i), "ONESf"], [psk(6)])
                P.act(lambda e, cb=cb, tc=tc: e.activation(out=cb[:, tc * 512:(tc + 1) * 512], in_=PS[6][:, :],
                                                           func=AF.Identity, bias=CST[:, 2:3], scale=1.0),
                      [psk(6), "CST2"], [("CB", c2i, tc)])
            for fh in range(2):
                wg = WB[wc[0] % 4]
                wgk = ("WB", wc[0] % 4)
                wc[0] += 1
                wu = WB[wc[0] % 4]
                wuk = ("WB", wc[0] % 4)
                wc[0] += 1
                P.dma("pool", wg[:, :, :], weg[l, g, ee, :, fh * 256:(fh + 1) * 256].rearrange("(k p) n -> p k n", p=128),
                      [], [wgk])
                P.dma("pool", wu[:, :, :], weu[l, g, ee, :, fh * 256:(fh + 1) * 256].rearrange("(k p) n -> p k n", p=128),
                      [], [wuk])
                for fb in range(2):
                    fc = fh * 2 + fb
                    for tc in range(2):
                        ci = cnt[0] % 2
                        cnt[0] += 1
                        pG, pU = ci, 2 + ci
                        for k in range(32):
                            P.pe(lambda e, wg=wg, fb=fb, tc=tc, k=k, pG=pG: e.matmul(
                                PS[pG][:, :], wg[:, k, fb * 128:(fb + 1) * 128], HT[:, k, tc * 512:(tc + 1) * 512],
                                start=(k == 0), stop=(k == 31)), [wgk], [psk(pG)])
                        for k in range(32):
                            P.pe(lambda e, wu=wu, fb=fb, tc=tc, k=k, pU=pU: e.matmul(
                                PS[pU][:, :], wu[:, k, fb * 128:(fb + 1) * 128], HT[:, k, tc * 512:(tc + 1) * 512],
                                start=(k == 0), stop=(k == 31)), [wuk], [psk(pU)])
                        sg, tu = SG[ci], TU[ci]
                        a3 = ai[0] % 3
                        ai[0] += 1
                        at = AT[a3]
                        P.act(lambda e, sg=sg, pG=pG: e.activation(out=sg[:, :], in_=PS[pG][:, :], func=AF.Silu,
                                                                   bias=CST[:, 2:3], scale=1.0),
                              [psk(pG), "CST2"], [("SG", ci)])
                        P.dve(lambda e, tu=tu, sg=sg, pU=pU: e.tensor_tensor(out=tu[:, :], in0=PS[pU][:, :], in1=sg[:, :],
                                                                             op=ALU.mult),
                              [psk(pU), ("SG", ci)], [("TU", ci)])
                        P.dve(lambda e, tu=tu, at=at, cb=cb, tc=tc: e.tensor_tensor(
                            out=at[:, :], in0=tu[:, :], in1=cb[:, tc * 512:(tc + 1) * 512], op=ALU.mult),
                            [("TU", ci), ("CB", c2i, tc)], [("AT", a3)])
                        P.dma("sp", AD[ex, fc][:, tc * 512:(tc + 1) * 512], at[:, :], [("AT", a3)], [ukey()])
        P.end_phase()

        if stop == "p1":
            P.begin_phase()
            DBG = P.sb("DBG", [128, 1024], BF16)
            P.dma("sp", DBG[:, :], AD[0, 0], [], ["DBG"])
            dump("d_out", DBG[:, :], ["DBG"])
            P.end_phase()
            return "stop"
        P.begin_phase()
        WB = [P.sb(f"WBd{i}", [128, 32, 256], BF16) for i in range(3)]
        AE = [P.sb(f"AE{i}", [128, 4, T], BF16) for i in range(3)]
        XKd = [P.sb(f"XKd{i}", [128, T], F32) for i in range(2)]
        XNd = [P.sb(f"XNd{i}", [128, T], F32) for i in range(2)]
        wed = I("w_exp_down")
        wc = [0]
        ac = [0]
        for c2 in range(16):
            for g in range(4):
                wb = WB[wc[0] % 3]
                wk = ("WB", wc[0] % 3)
                wc[0] += 1
                P.dma("pool", wb[:, :, :],
                      wed[l, g, :, :, c2 * 256:(c2 + 1) * 256].rearrange("e (fc p) n -> p (e fc) n", p=128), [], [wk])
                for ee in range(8):
                    ex = g * 8 + ee
                    a3 = ac[0] % 3
                    ac[0] += 1
                    ae = AE[a3]
                    P.dma("sp", ae[:, :, :], AD[ex].rearrange("f p t -> p f t"), [], [("AE", a3)])
                    for fc in range(4):
                        for cc in range(2):
                            for tc in range(2):
                                pb = cc * 2 + tc
                                first = (ex == 0 and fc == 0)
                                last = (ex == 31 and fc == 3)
                                P.pe(lambda e, wb=wb, ee=ee, fc=fc, cc=cc, tc=tc, pb=pb, ae=ae, first=first, last=last:
                                     e.matmul(PS[pb][:, :], wb[:, ee * 4 + fc, cc * 128:(cc + 1) * 128],
                                              ae[:, fc, tc * 512:(tc + 1) * 512], start=first, stop=last),
                                     [wk, ("AE", a3)], [psk(pb)])
            for cc in range(2):
                kc = c2 * 2 + cc
                x2 = kc % 2
                P.dma("sp", XKd[x2][:, :], XD[kc], [], [("XK", x2)])
                for tc in range(2):
                    pb = cc * 2 + tc
                    P.dve(lambda e, pb=pb, x2=x2, kc=kc, tc=tc, l=l: e.scalar_tensor_tensor(
                        out=XNd[x2][:, tc * 512:(tc + 1) * 512], in0=PS[pb][:, :], scalar=MOD[l][:, 160 + kc:161 + kc],
                        in1=XKd[x2][:, tc * 512:(tc + 1) * 512], op0=ALU.mult, op1=ALU.add),
                        [psk(pb), ("XK", x2)], [("XN", x2, tc)])
                P.dma("sp", XD[kc], XNd[x2][:, :], [("XN", x2, 0), ("XN", x2, 1)], [("XN", x2, 0), ("XN", x2, 1)])
        P.end_phase()

    for l in range(n_layers):
        if layer(l) == "stop":
            return finish()

    norm_stage(xsrc_xd, None, None, "final", 0)
    return finish()


def _consts():
    idf = np.eye(128, dtype=np.float32)
    tri = (np.arange(128)[:, None] <= np.arange(128)[None, :]).astype(np.float32)
    cmask = np.where(np.arange(128)[None, :] >= np.arange(128)[:, None], 0.0, NEG).astype(np.float32)
    bmask = np.zeros((128, 5, 128), np.float32)
    kk = (np.arange(128) >= 64)[:, None]
    qq = (np.arange(128) >= 64)[None, :]
    bmask[:, 0, :] = np.where((~kk) & qq, NEG, 0.0)
    bmask[:, 4, :] = np.where(kk & (~qq), NEG, 0.0)
    return {
        "c_idb": idf.astype(ml_dtypes.bfloat16), "c_idf": idf, "c_tri": tri, "c_cmask": cmask,
        "c_bmask": bmask.reshape(128, 640),
    }


def _rel_idx():
    k = np.arange(128)[:, None, None]
    a = np.arange(5)[None, :, None]
    q = np.arange(128)[None, None, :]
    ext = q - k + 127 + 128 * (4 - a)
    return np.minimum(ext + 1, 256).reshape(128, 640)


_REL_IDX = _rel_idx()


def prep_inputs(inp):
    f = lambda a: np.ascontiguousarray(np.asarray(a, dtype=np.float32))
    x = np.asarray(inp["x"], np.float32)
    c = np.asarray(inp["c"], np.float32)
    L = 2
    vt = lambda v: f(v.reshape(-1, 128).T)
    shared = {
        "cT_all": f(c.reshape(4, 32, 128).transpose(2, 1, 0).reshape(128, 128)),
        "gmixT": f(np.stack([vt(inp["norm_mix_g"][l]) for l in range(L)])),
        "gffnT": f(np.stack([vt(inp["norm_ffn_g"][l]) for l in range(L)])),
        "gfinT": vt(np.asarray(inp["final_norm_g"])),
        "w_in": f(inp["w_in"]),
        "bf_t": f(np.broadcast_to(np.asarray(inp["b_forget"])[:, None, :], (L, 128, 16))),
        "relT": f(np.asarray(inp["rel_bias"])[:, :, _REL_IDX]),
        "w_branch_a": f(inp["w_branch_a"]), "w_branch_b": f(inp["w_branch_b"]),
        "w_gate": f(inp["w_gate"]),
        "bgT": f(np.stack([vt(np.asarray(inp["b_gate"])[l]) for l in range(L)])),
        "w_out": f(inp["w_out"]),
        "w_r": f(np.concatenate([np.asarray(inp["w_router_group"]), np.asarray(inp["w_router_expert"])], axis=2)),
        "br_t": f(np.broadcast_to(np.concatenate([np.asarray(inp["b_router_group"]),
                                                  np.asarray(inp["b_router_expert"])], axis=1)[:, None, :],
                                  (L, 128, 36))),
        "w_exp_gate": f(inp["w_exp_gate"]), "w_exp_up": f(inp["w_exp_up"]), "w_exp_down": f(inp["w_exp_down"]),
    }
    shared.update(_consts())
    ada_w = np.asarray(inp["ada_w"], np.float32)
    ada_b = np.asarray(inp["ada_b"], np.float32)
    maps = []
    flat = {}
    if SHARD_W:
        for nm, nlead in WSHARD.items():
            a = shared.pop(nm)
            npieces = int(np.prod(a.shape[:nlead]))
            flat[nm] = a.reshape(npieces, 8, -1, a.shape[-1])
    for cid in range(8):
        b, half = cid // 2, cid % 2
        m = dict(shared)
        if SHARD_W:
            for nm in WSHARD:
                m[nm] = np.ascontiguousarray(flat[nm][:, cid])
        m["xT"] = f(x[b, half * T:(half + 1) * T, :].T)
        m["ada_w_s"] = f(ada_w[:, :, cid * 3072:(cid + 1) * 3072])
        m["ada_bT"] = f(ada_b[:, cid * 3072:(cid + 1) * 3072].reshape(L, 24, 128).transpose(0, 2, 1))
        bs = np.zeros((128, 4), np.float32)
        bs[:, b] = 1.0
        m["bsel"] = bs
        m["pen"] = np.full((128, 1), 0.0 if half == 1 else NEG, np.float32)
        maps.append(m)
    return maps


def used_inputs(nc):
    names = set()
    for alloc in nc.allocations:
        if isinstance(alloc, mybir.MemoryLocationSet) and alloc.kind == "ExternalInput":
            names.add(alloc.memorylocations[0].name)
    return names


def kernel(**inputs):
    nc = build()
    used = used_inputs(nc)
    maps = [{k: v for k, v in m.items() if k in used} for m in prep_inputs(inputs)]
    res = run_bass_kernel_spmd(nc, maps, core_ids=list(range(8)))
    out = np.empty((4, 2048, D), np.float32)
    for cid in range(8):
        b, half = cid // 2, cid % 2
        out[b, half * T:(half + 1) * T, :] = res.results[cid]["outT"].T
    return out
```
